# Optimizing a Trainium2 kernel written in Bass

```python
import math
import jax, jax.numpy as jnp
from jax import lax
import numpy as np

D_MODEL = 1024
BATCH = 4
SEQ = 8192
DEPTH = 2

CHUNK = 64
Q_BLOCK = 128
N_EVEN = (DEPTH + 1) // 2
N_ODD = DEPTH // 2

A_HEADS = 4
A_DK = 128
A_DV = 128
CONV_WIDTH = 4
B_HEADS = 4
B_DH = 128
IDX_HEADS = 8
IDX_DIM = 64
TOPK_MAX = 256
C_HEADS = 4
C_DH = 128
D_HEADS = 4
D_DK = 128
D_DV = 128
REL_BUCKETS = 32
REL_MAX_DIST = 128
D_FF = ((8 * D_MODEL // 3 + 255) // 256) * 256

A_QKV_W = 2 * A_HEADS * A_DK + A_HEADS * A_DV
B_W = B_HEADS * B_DH
EVEN_WIDTHS = (A_QKV_W, A_HEADS * A_DV, A_HEADS, A_HEADS, B_W, B_W, B_W, IDX_HEADS * IDX_DIM, IDX_DIM, IDX_HEADS)
EVEN_IN = A_QKV_W + A_HEADS * A_DV + 2 * A_HEADS + 3 * B_W + IDX_HEADS * IDX_DIM + IDX_DIM + IDX_HEADS
MIX_EVEN = A_HEADS * A_DV + B_HEADS * B_DH
C_W = C_HEADS * C_DH
ODD_WIDTHS = (C_W, C_W, C_W, D_HEADS * D_DK, D_HEADS * D_DK, D_HEADS * D_DV, D_HEADS * D_DV)
ODD_IN = 3 * C_W + 2 * D_HEADS * D_DK + 2 * D_HEADS * D_DV
MIX_ODD = C_HEADS * C_DH + D_HEADS * D_DV

kernel_name = "hybrid_deltanet_dsa_stickbreak_hgrn2_trunk"


def _split(t, widths):
    out = []
    start = 0
    for w in widths:
        out.append(t[..., start:start + w])
        start += w
    return out


def _heads(t, n):
    return t.reshape(t.shape[0], t.shape[1], n, -1)


def rms_norm(x, g, eps=1e-6):
    xf = x.astype(jnp.float32)
    y = xf * lax.rsqrt(jnp.mean(xf * xf, axis=-1, keepdims=True) + eps)
    return (y * g.astype(jnp.float32)).astype(x.dtype)


def causal_conv(x, w):
    width = w.shape[0]
    s = x.shape[1]
    xp = jnp.pad(x, ((0, 0), (width - 1, 0), (0, 0)))
    out = xp[:, 0:s] * w[0]
    for j in range(1, width):
        out = out + xp[:, j:j + s] * w[j]
    return out


def t5_bucket(rel):
    nb = REL_BUCKETS // 2
    max_exact = nb // 2
    ret = jnp.where(rel > 0, nb, 0)
    n = jnp.abs(rel)
    large = max_exact + (jnp.log(jnp.maximum(n, 1).astype(jnp.float32) / max_exact)
                         / math.log(REL_MAX_DIST / max_exact) * (nb - max_exact)).astype(jnp.int32)
    large = jnp.minimum(large, nb - 1)
    return ret + jnp.where(n < max_exact, n, large)


def _to_chunks(t):
    b, s, h, d = t.shape
    return t.reshape(b, s // CHUNK, CHUNK, h, d).transpose(0, 3, 1, 2, 4)


def _from_scan(o):
    n, b, h, c, d = o.shape
    return o.transpose(1, 0, 3, 2, 4).reshape(b, n * c, h, d)


def _l2norm(t, eps=1e-6):
    return t * lax.rsqrt(jnp.sum(t * t, axis=-1, keepdims=True) + eps)


def gated_deltanet(q, k, v, a, b, a_log, dt_bias):
    bsz, _, h, dk = q.shape
    dv = v.shape[-1]
    q = _l2norm(q) * (dk ** -0.5)
    k = _l2norm(k)
    beta = jax.nn.sigmoid(b)
    g = -jnp.exp(a_log) * jax.nn.softplus(a + dt_bias)
    q, k, v = _to_chunks(q), _to_chunks(k), _to_chunks(v)
    beta = _to_chunks(beta[..., None])[..., 0]
    gc = jnp.cumsum(_to_chunks(g[..., None])[..., 0], axis=-1)
    tri = jnp.tril(jnp.ones((CHUNK, CHUNK), bool))
    strict = jnp.tril(jnp.ones((CHUNK, CHUNK), bool), k=-1)
    decay = jnp.exp(jnp.where(tri, gc[..., :, None] - gc[..., None, :], -jnp.inf))
    kk = jnp.einsum('bhncd,bhnjd->bhncj', k, k)
    m = jnp.eye(CHUNK, dtype=q.dtype) + jnp.where(strict, beta[..., None] * kk * decay, 0.0)
    rhs = jnp.concatenate([v * beta[..., None], k * (beta * jnp.exp(gc))[..., None]], axis=-1)
    sol = lax.linalg.triangular_solve(m, rhs, left_side=True, lower=True, unit_diagonal=True)
    u, w = sol[..., :dv], sol[..., dv:]
    qk = jnp.einsum('bhncd,bhnjd->bhncj', q, k) * decay
    q_dec = q * jnp.exp(gc)[..., None]
    k_dec = k * jnp.exp(gc[..., -1:] - gc)[..., None]
    g_last = jnp.exp(gc[..., -1])

    def step(state, xs):
        q_c, qk_c, u_c, w_c, kd_c, gl_c = xs
        v_new = u_c - jnp.einsum('bhcd,bhdv->bhcv', w_c, state)
        o = jnp.einsum('bhcd,bhdv->bhcv', q_c, state) + jnp.einsum('bhcj,bhjv->bhcv', qk_c, v_new)
        state = state * gl_c[..., None, None] + jnp.einsum('bhcd,bhcv->bhdv', kd_c, v_new)
        return state, o

    xs = tuple(jnp.moveaxis(t, 2, 0) for t in (q_dec, qk, u, w, k_dec, g_last))
    s0 = jnp.zeros((bsz, h, dk, dv), q.dtype)
    _, o = lax.scan(step, s0, xs)
    return _from_scan(o)


def dsa_attention(q, k, v, qi, ki, wi, rel_table):
    bsz, s, h, dh = q.shape
    k_sel = min(TOPK_MAX, s // 4)
    nblk = s // Q_BLOCK
    key_pos = jnp.arange(s)
    wi = wi * (IDX_HEADS ** -0.5)

    def block(start):
        qb = lax.dynamic_slice_in_dim(q, start, Q_BLOCK, axis=1)
        qib = lax.dynamic_slice_in_dim(qi, start, Q_BLOCK, axis=1)
        wib = lax.dynamic_slice_in_dim(wi, start, Q_BLOCK, axis=1)
        qpos = start + jnp.arange(Q_BLOCK)
        limit = (qpos // CHUNK + 1) * CHUNK
        admissible = key_pos[None, :] < limit[:, None]
        score = jnp.einsum('bthd,bsd->bths', qib, ki) * (IDX_DIM ** -0.5)
        score = jnp.einsum('bths,bth->bts', jax.nn.relu(score), wib)
        score = jnp.where(admissible[None], score, -jnp.inf)
        _, idx = lax.top_k(score, k_sel)
        k_g = jax.vmap(lambda kk, ii: kk[ii])(k, idx)
        v_g = jax.vmap(lambda vv, ii: vv[ii])(v, idx)
        valid = idx < limit[None, :, None]
        bias = rel_table[t5_bucket(idx - qpos[None, :, None])]
        logits = jnp.einsum('bthd,btkhd->bthk', qb, k_g) * (dh ** -0.5) + bias.transpose(0, 1, 3, 2)
        logits = jnp.where(valid[:, :, None, :], logits, -jnp.inf)
        p = jax.nn.softmax(logits, axis=-1)
        return jnp.einsum('bthk,btkhd->bthd', p, v_g)

    out = lax.map(block, jnp.arange(nblk) * Q_BLOCK)
    return out.transpose(1, 0, 2, 3, 4).reshape(bsz, s, h, dh)


def stick_breaking(q, k, v):
    bsz, s, h, dh = q.shape
    nblk = s // Q_BLOCK
    kh = k.transpose(0, 2, 1, 3)
    vh = v.transpose(0, 2, 1, 3)
    key_pos = jnp.arange(s)

    def block(start):
        qb = lax.dynamic_slice_in_dim(q, start, Q_BLOCK, axis=1)
        qpos = start + jnp.arange(Q_BLOCK)
        causal = key_pos[None, :] < qpos[:, None]
        z = jnp.einsum('bthd,bhsd->bhts', qb, kh) * (dh ** -0.5)
        log_1mb = jnp.where(causal, jax.nn.log_sigmoid(-z), 0.0)
        rest = lax.cumsum(log_1mb, axis=3, reverse=True) - log_1mb
        logw = jnp.where(causal, jax.nn.log_sigmoid(z) + rest, -jnp.inf)
        return jnp.einsum('bhts,bhsd->bthd', jnp.exp(logw), vh)

    out = lax.map(block, jnp.arange(nblk) * Q_BLOCK)
    return out.transpose(1, 0, 2, 3, 4).reshape(bsz, s, h, dh)


def hgrn2(q, f_raw, i, lb):
    bsz, _, h, dk = q.shape
    dv = i.shape[-1]
    log_f = jnp.logaddexp(jnp.log(lb), jnp.log1p(-lb) + jax.nn.log_sigmoid(f_raw))
    key = (1.0 - lb) * jax.nn.sigmoid(-f_raw)
    q, key, i = _to_chunks(q), _to_chunks(key), _to_chunks(i)
    gc = jnp.cumsum(_to_chunks(log_f), axis=3)
    tri = jnp.tril(jnp.ones((CHUNK, CHUNK), bool))[:, :, None]

    def step(state, xs):
        q_c, k_c, v_c, g_c = xs
        diff = g_c[:, :, :, None, :] - g_c[:, :, None, :, :]
        decay = jnp.exp(jnp.where(tri, diff, -jnp.inf))
        attn = jnp.einsum('bhid,bhijd,bhjd->bhij', q_c, decay, k_c)
        o = jnp.einsum('bhid,bhdv->bhiv', q_c * jnp.exp(g_c), state) + jnp.einsum('bhij,bhjv->bhiv', attn, v_c)
        g_last = g_c[:, :, -1:, :]
        state = jnp.exp(g_last)[:, :, 0, :, None] * state + jnp.einsum('bhjd,bhjv->bhdv', k_c * jnp.exp(g_last - g_c), v_c)
        return state, o

    xs = tuple(jnp.moveaxis(t, 2, 0) for t in (q, key, i, gc))
    s0 = jnp.zeros((bsz, h, dk, dv), q.dtype)
    _, o = lax.scan(step, s0, xs)
    return _from_scan(o)


def even_mixer(y, w_in, conv_w, a_log, dt_bias, a_norm_g, rel_table, w_out):
    f32 = jnp.float32
    p = (y @ w_in).astype(f32)
    qkv_a, z_a, a_a, b_a, q_b, k_b, v_b, q_i, k_i, w_i = _split(p, EVEN_WIDTHS)
    qkv_a = jax.nn.silu(causal_conv(qkv_a, conv_w.astype(f32)))
    q_a, k_a, v_a = _split(qkv_a, (A_HEADS * A_DK, A_HEADS * A_DK, A_HEADS * A_DV))
    o_a = gated_deltanet(_heads(q_a, A_HEADS), _heads(k_a, A_HEADS), _heads(v_a, A_HEADS),
                         a_a, b_a, a_log.astype(f32), dt_bias.astype(f32))
    o_a = rms_norm(o_a, a_norm_g) * jax.nn.silu(_heads(z_a, A_HEADS))
    o_b = dsa_attention(_heads(q_b, B_HEADS), _heads(k_b, B_HEADS), _heads(v_b, B_HEADS),
                        _heads(q_i, IDX_HEADS), k_i, w_i, rel_table.astype(f32))
    bsz, s = y.shape[0], y.shape[1]
    cat = jnp.concatenate([o_a.reshape(bsz, s, -1), o_b.reshape(bsz, s, -1)], axis=-1)
    return cat.astype(y.dtype) @ w_out


def odd_mixer(y, w_in, lb, d_norm_g, w_out):
    f32 = jnp.float32
    p = (y @ w_in).astype(f32)
    q_c, k_c, v_c, q_d, f_d, i_d, g_d = _split(p, ODD_WIDTHS)
    o_c = stick_breaking(_heads(q_c, C_HEADS), _heads(k_c, C_HEADS), _heads(v_c, C_HEADS))
    lb_h = lb.astype(f32).reshape(D_HEADS, D_DK)
    o_d = hgrn2(jax.nn.silu(_heads(q_d, D_HEADS)), _heads(f_d, D_HEADS), _heads(i_d, D_HEADS), lb_h)
    o_d = rms_norm(o_d, d_norm_g) * jax.nn.silu(_heads(g_d, D_HEADS))
    bsz, s = y.shape[0], y.shape[1]
    cat = jnp.concatenate([o_c.reshape(bsz, s, -1), o_d.reshape(bsz, s, -1)], axis=-1)
    return cat.astype(y.dtype) @ w_out


def swiglu(y, wg, wu, wd):
    return (jax.nn.silu(y @ wg) * (y @ wu)) @ wd


def setup_inputs(seed: int = 0) -> dict:
    key = jax.random.key(seed)
    ks = jax.random.split(key, 16)
    f32 = jnp.float32

    def normal(k, shape, scale):
        return jax.random.normal(k, shape, f32) * scale

    x = normal(ks[0], (BATCH, SEQ, D_MODEL), 1.0)
    norm_g = 1.0 + normal(ks[1], (DEPTH, 4, D_MODEL), 0.02)
    w_in_even = normal(ks[2], (N_EVEN, D_MODEL, EVEN_IN), D_MODEL ** -0.5)
    conv_w_even = normal(ks[3], (N_EVEN, CONV_WIDTH, A_QKV_W), CONV_WIDTH ** -0.5)
    a_log_even = jnp.log(jax.random.uniform(ks[4], (N_EVEN, A_HEADS), f32, 1.0, 16.0))
    dt = jnp.exp(jax.random.uniform(ks[5], (N_EVEN, A_HEADS), f32, math.log(1e-3), math.log(1e-1)))
    dt_bias_even = dt + jnp.log(-jnp.expm1(-dt))
    a_norm_even = 1.0 + normal(ks[6], (N_EVEN, A_DV), 0.02)
    w_out_even = normal(ks[7], (N_EVEN, MIX_EVEN, D_MODEL), MIX_EVEN ** -0.5)
    rel_bias = normal(ks[8], (REL_BUCKETS, B_HEADS), 0.5)
    w_in_odd = normal(ks[9], (N_ODD, D_MODEL, ODD_IN), D_MODEL ** -0.5)
    lb_logits = normal(ks[10], (DEPTH, D_HEADS * D_DK), 0.1)
    d_norm_odd = 1.0 + normal(ks[11], (N_ODD, D_DV), 0.02)
    w_out_odd = normal(ks[12], (N_ODD, MIX_ODD, D_MODEL), MIX_ODD ** -0.5)
    w_gate = normal(ks[13], (DEPTH, D_MODEL, D_FF), D_MODEL ** -0.5)
    w_up = normal(ks[14], (DEPTH, D_MODEL, D_FF), D_MODEL ** -0.5)
    w_down = normal(ks[15], (DEPTH, D_FF, D_MODEL), D_FF ** -0.5)
    return {"x": x, "norm_g": norm_g, "w_in_even": w_in_even, "conv_w_even": conv_w_even,
            "a_log_even": a_log_even, "dt_bias_even": dt_bias_even, "a_norm_even": a_norm_even,
            "w_out_even": w_out_even, "rel_bias": rel_bias, "w_in_odd": w_in_odd,
            "lb_logits": lb_logits, "d_norm_odd": d_norm_odd, "w_out_odd": w_out_odd,
            "w_gate": w_gate, "w_up": w_up, "w_down": w_down}


def reference(x, norm_g, w_in_even, conv_w_even, a_log_even, dt_bias_even, a_norm_even,
              w_out_even, rel_bias, w_in_odd, lb_logits, d_norm_odd, w_out_odd,
              w_gate, w_up, w_down):
    lb_all = jnp.cumsum(jax.nn.softmax(lb_logits.astype(jnp.float32), axis=0), axis=0)
    lb_all = lb_all - lb_all[:1]
    h = x
    for l in range(DEPTH):
        y = rms_norm(h, norm_g[l, 0])
        if l % 2 == 0:
            e = l // 2
            y = even_mixer(y, w_in_even[e], conv_w_even[e], a_log_even[e], dt_bias_even[e],
                           a_norm_even[e], rel_bias, w_out_even[e])
        else:
            o = l // 2
            y = odd_mixer(y, w_in_odd[o], lb_all[l], d_norm_odd[o], w_out_odd[o])
        h = h + rms_norm(y, norm_g[l, 1])
        y = swiglu(rms_norm(h, norm_g[l, 2]), w_gate[l], w_up[l], w_down[l])
        h = h + rms_norm(y, norm_g[l, 3])
    return h
```

```python
import math
from contextlib import ExitStack
import numpy as np
import concourse.bass as bass
import concourse.mybir as mybir

F32 = mybir.dt.float32
BF16 = mybir.dt.bfloat16
I32 = mybir.dt.int32
AF = mybir.ActivationFunctionType
ALU = mybir.AluOpType
AX = mybir.AxisListType

ENGS = ("pe", "act", "dve", "pool", "sp")
NSLOT = 8


def _box(ap):
    t = ap.tensor
    pat = ap.ap
    off = int(ap.offset)
    sp = str(ap.space)
    if "DRAM" in sp.upper() or "HBM" in sp.upper():
        ext = sum((c - 1) * abs(s) for s, c in pat)
        return (t.name, 0, 1, off, off + ext + 1)
    row = 1
    for d in t.shape[1:]:
        row *= int(d)
    if "PSUM" in sp.upper():
        return (t.name, 0, 128, 0, row)
    pstep, pcnt = pat[0]
    p0 = off // row
    f0 = off % row
    if pstep == 0:
        pcnt = 1
    ext = sum((c - 1) * abs(s) for s, c in pat[1:])
    return (t.name, p0, p0 + pcnt, f0, f0 + ext + 1)


def _ovl(a, b):
    return a[1] < b[2] and b[1] < a[2] and a[3] < b[4] and b[3] < a[4]


def _inside(a, b):
    return a[1] >= b[1] and a[2] <= b[2] and a[3] >= b[3] and a[4] <= b[4]


class Prog:
    def __init__(self, nc, track_dram=()):
        self.nc = nc
        self.ops = []
        self.W = {}
        self.R = {}
        self.track_dram = set(track_dram)

    def _boxes(self, aps):
        out = []
        for ap in aps:
            if ap is None:
                continue
            if isinstance(ap, tuple):
                out.append(ap)
                continue
            b = _box(ap)
            sp = str(ap.space).upper()
            if ("DRAM" in sp or "HBM" in sp) and b[0] not in self.track_dram:
                continue
            out.append(b)
        return out

    def op(self, eng, fn, reads=(), writes=(), dma=False):
        oid = len(self.ops)
        deps = set()
        rb = self._boxes(reads)
        wb = self._boxes(writes)
        for b in rb:
            for (wbx, wid) in self.W.get(b[0], ()):
                if _ovl(b, wbx):
                    deps.add(wid)
        for b in wb:
            for (wbx, wid) in self.W.get(b[0], ()):
                if _ovl(b, wbx):
                    deps.add(wid)
            for (rbx, rid) in self.R.get(b[0], ()):
                if _ovl(b, rbx):
                    deps.add(rid)
        if eng == "pe" and not dma:
            deps = {d for d in deps if not (self.ops[d]["eng"] == "pe" and not self.ops[d]["dma"])}
        self.ops.append(dict(eng=eng, fn=fn, dma=dma, deps=deps))
        for b in wb:
            lst = self.W.setdefault(b[0], [])
            lst[:] = [(x, i) for (x, i) in lst if not _inside(x, b)]
            lst.append((b, oid))
            rl = self.R.get(b[0])
            if rl:
                rl[:] = [(x, i) for (x, i) in rl if not _inside(x, b)]
        for b in rb:
            rl = self.R.setdefault(b[0], [])
            if not dma:
                rl[:] = [(x, i) for (x, i) in rl
                         if not (x == b and self.ops[i]["eng"] == eng and not self.ops[i]["dma"])]
            rl.append((b, oid))
        return oid

    def dma(self, q, out, in_, **kw):
        return self.op(q, lambda e: e.dma_start(out=out, in_=in_, **kw), [in_], [out], dma=True)

    def mm(self, out, lhsT, rhs, start=True, stop=True, **kw):
        return self.op("pe", lambda e: e.matmul(out, lhsT, rhs, start=start, stop=stop, **kw),
                       [lhsT, rhs], [out])

    def tr(self, out, in_, ident):
        return self.op("pe", lambda e: e.transpose(out, in_, ident), [in_, ident], [out])

    def act(self, out, in_, func, bias=None, scale=1.0, accum_out=None, eng="act"):
        rd = [in_]
        if bias is not None and not isinstance(bias, (int, float)):
            rd.append(bias)
        if not isinstance(scale, (int, float)):
            rd.append(scale)
        wr = [out]
        if accum_out is not None:
            wr.append(accum_out)
        kw = {}
        if bias is not None:
            kw["bias"] = bias
        if accum_out is not None:
            kw["accum_out"] = accum_out
        return self.op(eng, lambda e: e.activation(out=out, in_=in_, func=func, scale=scale, **kw), rd, wr)

    def tt(self, eng, out, in0, in1, op):
        return self.op(eng, lambda e: e.tensor_tensor(out=out, in0=in0, in1=in1, op=op), [in0, in1], [out])

    def ts(self, eng, out, in0, s1, op0, s2=None, op1=None, accum_out=None):
        rd = [in0]
        for s in (s1, s2):
            if s is not None and not isinstance(s, (int, float)):
                rd.append(s)
        wr = [out]
        if accum_out is not None:
            wr.append(accum_out)
        kw = {}
        if op1 is not None:
            kw["op1"] = op1
        if accum_out is not None:
            kw["accum_out"] = accum_out
        return self.op(eng, lambda e: e.tensor_scalar(out=out, in0=in0, scalar1=s1, scalar2=s2, op0=op0, **kw), rd, wr)

    def stt(self, out, in0, scalar, in1, op0, op1, eng="dve"):
        rd = [in0, in1]
        if not isinstance(scalar, (int, float)):
            rd.append(scalar)
        return self.op(eng, lambda e: e.scalar_tensor_tensor(out=out, in0=in0, scalar=scalar, in1=in1, op0=op0, op1=op1), rd, [out])

    def copy(self, eng, out, in_):
        if eng == "act":
            return self.op(eng, lambda e: e.copy(out=out, in_=in_), [in_], [out])
        return self.op(eng, lambda e: e.tensor_copy(out=out, in_=in_), [in_], [out])

    def memset(self, eng, ap, val):
        return self.op(eng, lambda e: e.memset(ap, val), [], [ap])

    def emit(self, stack):
        nc = self.nc
        ops = self.ops
        need = [False] * len(ops)
        for o in ops:
            for d in o["deps"]:
                need[d] = True
        sem = {e: stack.enter_context(nc.semaphore("s_" + e)) for e in ENGS}
        dsem = {e: [stack.enter_context(nc.semaphore("d_%s%d" % (e, i))) for i in range(NSLOT)]
                for e in ("sp", "act", "pool")}
        cnt = {e: 0 for e in ENGS}
        dcnt = {e: 0 for e in dsem}
        sig = [None] * len(ops)
        prevslot = [None] * len(ops)
        for i, o in enumerate(ops):
            if o["dma"]:
                q = o["eng"]
                n = dcnt[q]
                dcnt[q] += 1
                sig[i] = (dsem[q][n % NSLOT], 16 * (n // NSLOT + 1))
                if n >= NSLOT:
                    prevslot[i] = (dsem[q][n % NSLOT], 16 * (n // NSLOT))
            elif need[i]:
                cnt[o["eng"]] += 1
                sig[i] = (sem[o["eng"]], cnt[o["eng"]])
        per = {e: [] for e in ENGS}
        for i, o in enumerate(ops):
            per[o["eng"]].append(i)
        finals = {q: [(dsem[q][s], 16 * ((dcnt[q] - 1 - s) // NSLOT + 1))
                      for s in range(min(NSLOT, dcnt[q]))] for q in dsem}
        block = stack.enter_context(nc.Block())

        def run(engname):
            def body(e):
                seen = {}
                for i in per[engname]:
                    o = ops[i]
                    waits = []
                    for d in sorted(o["deps"]):
                        waits.append(sig[d])
                    if prevslot[i] is not None:
                        waits.append(prevslot[i])
                    for (s, v) in waits:
                        k = id(s)
                        if seen.get(k, 0) >= v:
                            continue
                        seen[k] = v
                        e.wait_ge(s, v)
                    ins = o["fn"](e)
                    if sig[i] is not None and (o["dma"] or need[i]):
                        ins.then_inc(sig[i][0], 16 if o["dma"] else 1)
                if engname in finals:
                    for (s, v) in finals[engname]:
                        if seen.get(id(s), 0) < v:
                            e.wait_ge(s, v)
            return body

        block.tensor(run("pe"))
        block.scalar(run("act"))
        block.vector(run("dve"))
        block.gpsimd(run("pool"))
        block.sync(run("sp"))

D = 1024
KC = D // 128
EPS = 1e-6


class Ctx:
    def __init__(self, track_dram=()):
        self.nc = bass.Bass("TRN2", target_bir_lowering=False)
        self.st = ExitStack()
        self.P = Prog(self.nc, track_dram=track_dram)
        self._n = 0

    def din(self, name, shape, dt=F32):
        return self.nc.dram_tensor(name, list(shape), dt, kind="ExternalInput").ap()

    def dout(self, name, shape, dt=F32):
        return self.nc.dram_tensor(name, list(shape), dt, kind="ExternalOutput").ap()

    def sb(self, name, shape, dt=F32):
        return self.st.enter_context(self.nc.sbuf_tensor(name, list(shape), dt))

    def ps(self, name, shape, dt=F32):
        return self.st.enter_context(self.nc.psum_tensor(name, list(shape), dt))

    def finish(self):
        self.P.emit(self.st)
        self.st.close()
        return self.nc


class Rot:
    def __init__(self, bufs):
        self.bufs = bufs
        self.i = 0

    def next(self):
        b = self.bufs[self.i % len(self.bufs)]
        self.i += 1
        return b


def load_weight_bf16(c, Wd, Wb, stage, gain=None, nsplit=1024):
    P = c.P
    K, N = Wd.shape
    i = 0
    for kc in range(K // 128):
        for c0 in range(0, N, nsplit):
            w = min(nsplit, N - c0)
            s = stage.next()
            P.dma(("sp", "act")[i % 2], s[:, :w], Wd[kc * 128:(kc + 1) * 128, c0:c0 + w])
            if gain is not None:
                if i % 2 == 0:
                    P.ts("dve", Wb[:, kc, c0:c0 + w], s[:, :w], gain[:, kc:kc + 1], ALU.mult)
                else:
                    P.act(Wb[:, kc, c0:c0 + w], s[:, :w], AF.Copy, scale=gain[:, kc:kc + 1])
            else:
                P.copy(("dve", "act")[i % 2], Wb[:, kc, c0:c0 + w], s[:, :w])
            i += 1


def emit_rstd(c, xt, nch, TT, sq, ones, ps_stat, rstd, dim):
    P = c.P
    P.act(sq[:, :nch, :TT], xt[:, :nch, :TT], AF.Square)
    for k in range(nch):
        P.mm(ps_stat[:, :TT], ones[:], sq[:, k, :TT], start=(k == 0), stop=(k == nch - 1))
    P.act(rstd[:, :TT], ps_stat[:, :TT], AF.Sqrt, bias=c.eps_t[:, 0:1], scale=1.0 / dim)
    P.op("dve", lambda e: e.reciprocal(out=rstd[:, :TT], in_=rstd[:, :TT]), [rstd[:, :TT]], [rstd[:, :TT]])


def setup_common(c):
    c.ones = c.sb("ones", [128, 128], BF16)
    c.P.memset("dve", c.ones[:], 1.0)
    c.eps_t = c.sb("eps_t", [128, 1])
    c.P.memset("dve", c.eps_t[:], EPS)


def emit_postnorm_res(c, yT, hT, g, outT, TT, sq, ps_stat, rstd):
    P = c.P
    emit_rstd(c, yT, KC, TT, sq, c.ones, ps_stat, rstd, D)
    for j in range(KC):
        P.stt(outT[:, j, :TT], yT[:, j, :TT], g[:, j:j + 1], rstd[:, :TT], ALU.mult, ALU.mult)
        P.tt("pool", outT[:, j, :TT], outT[:, j, :TT], hT[:, j, :TT], ALU.add)


def build_inproj(T, N, TT=512):
    c = Ctx()
    P = c.P
    xT = c.din("xT", [D, T])
    gd = c.din("g", [128, KC])
    Wd = c.din("W", [D, N])
    pT = c.dout("pT", [N, T])
    setup_common(c)
    g = c.sb("g_sb", [128, KC])
    P.dma("sp", g[:], gd)
    Wb = c.sb("Wb", [128, KC, N], BF16)
    stage = Rot([c.sb("wst%d" % i, [128, 1024]) for i in range(2)])
    load_weight_bf16(c, Wd, Wb, stage, gain=g)
    xbuf = Rot([c.sb("x%d" % i, [128, KC, TT]) for i in range(2)])
    xn = Rot([c.sb("xn%d" % i, [128, KC, TT], BF16) for i in range(2)])
    sq = c.sb("sq", [128, KC, TT], BF16)
    rstd = c.sb("rstd", [128, TT])
    ps_stat = c.ps("ps_stat", [128, 512])
    psb = Rot([c.ps("ps%d" % i, [128, 512]) for i in range(4)])
    ob = Rot([c.sb("ob%d" % i, [128, TT]) for i in range(4)])
    xTv = xT.rearrange("(k p) t -> p k t", p=128)
    nt = T // TT
    nchunks = (N + 127) // 128

    def norm(i):
        xb = xbuf.next()
        P.dma("sp", xb[:], xTv[:, :, i * TT:(i + 1) * TT])
        emit_rstd(c, xb, KC, TT, sq, c.ones, ps_stat, rstd, D)
        xo = xn.next()
        for k in range(KC):
            P.tt(("dve", "pool")[k % 2], xo[:, k, :], xb[:, k, :], rstd[:, :TT], ALU.mult)
        return xo

    cur = norm(0)
    for i in range(nt):
        nxt = norm(i + 1) if i + 1 < nt else None
        for j in range(nchunks):
            w = min(128, N - j * 128)
            pb = psb.next()
            for k in range(KC):
                P.mm(pb[:w, :TT], Wb[:, k, j * 128:j * 128 + w], cur[:, k, :], start=(k == 0), stop=(k == KC - 1))
            o = ob.next()
            P.copy(("act", "dve")[j % 2], o[:w, :], pb[:w, :TT])
            P.dma(("sp", "pool")[j % 2], pT[j * 128:j * 128 + w, i * TT:(i + 1) * TT], o[:w, :])
        cur = nxt
    return c.finish()


def build_outproj(T, TT=512):
    c = Ctx()
    P = c.P
    catT = c.din("catT", [D, T])
    hTd = c.din("hT", [D, T])
    gd = c.din("g", [128, KC])
    Wd = c.din("W", [D, D])
    oT = c.dout("oT", [D, T])
    setup_common(c)
    g = c.sb("g_sb", [128, KC])
    P.dma("sp", g[:], gd)
    Wb = c.sb("Wb", [128, KC, D], BF16)
    stage = Rot([c.sb("wst%d" % i, [128, 1024]) for i in range(2)])
    load_weight_bf16(c, Wd, Wb, stage)
    cbuf = Rot([c.sb("c%d" % i, [128, KC, TT]) for i in range(2)])
    cb16 = Rot([c.sb("cb%d" % i, [128, KC, TT], BF16) for i in range(2)])
    hbuf = Rot([c.sb("h%d" % i, [128, KC, TT]) for i in range(2)])
    ybuf = c.sb("y", [128, KC, TT])
    obuf = Rot([c.sb("o%d" % i, [128, KC, TT]) for i in range(2)])
    sq = c.sb("sq", [128, KC, TT], BF16)
    rstd = c.sb("rstd", [128, TT])
    ps_stat = c.ps("ps_stat", [128, 512])
    psb = Rot([c.ps("ps%d" % i, [128, 512]) for i in range(4)])
    cv = catT.rearrange("(k p) t -> p k t", p=128)
    hv = hTd.rearrange("(k p) t -> p k t", p=128)
    ov = oT.rearrange("(k p) t -> p k t", p=128)
    for i in range(T // TT):
        cb = cbuf.next()
        P.dma("sp", cb[:], cv[:, :, i * TT:(i + 1) * TT])
        hb = hbuf.next()
        P.dma("act", hb[:], hv[:, :, i * TT:(i + 1) * TT])
        c16 = cb16.next()
        for k in range(KC):
            P.copy(("dve", "pool")[k % 2], c16[:, k, :], cb[:, k, :])
        for j in range(KC):
            pb = psb.next()
            for k in range(KC):
                P.mm(pb[:, :TT], Wb[:, k, j * 128:(j + 1) * 128], c16[:, k, :], start=(k == 0), stop=(k == KC - 1))
            P.copy(("act", "dve")[j % 2], ybuf[:, j, :], pb[:, :TT])
        ob = obuf.next()
        emit_postnorm_res(c, ybuf, hb, g, ob, TT, sq, ps_stat, rstd)
        P.dma("pool", ov[:, :, i * TT:(i + 1) * TT], ob[:])
    return c.finish()


def build_ffn(T, DFF, TT=256):
    c = Ctx()
    P = c.P
    JC = DFF // 128
    hTd = c.din("hT", [D, T])
    g2d = c.din("g2", [128, KC])
    g3d = c.din("g3", [128, KC])
    Wgd = c.din("Wg", [D, DFF])
    Wud = c.din("Wu", [D, DFF])
    Wdd = c.din("Wd", [DFF, D])
    oT = c.dout("oT", [D, T])
    setup_common(c)
    g2 = c.sb("g2_sb", [128, KC])
    g3 = c.sb("g3_sb", [128, KC])
    P.dma("sp", g2[:], g2d)
    P.dma("sp", g3[:], g3d)
    Wg = c.sb("Wg_sb", [128, KC, DFF], BF16)
    Wu = c.sb("Wu_sb", [128, KC, DFF], BF16)
    Wdn = c.sb("Wdn", [128, JC, D], BF16)
    stage = Rot([c.sb("wst%d" % i, [128, 1024]) for i in range(2)])
    load_weight_bf16(c, Wgd, Wg, stage, gain=g2)
    load_weight_bf16(c, Wud, Wu, stage, gain=g2)
    load_weight_bf16(c, Wdd, Wdn, stage)
    hbuf = Rot([c.sb("h%d" % i, [128, KC, TT]) for i in range(2)])
    xn = c.sb("xn", [128, KC, TT], BF16)
    at = c.sb("at", [128, JC, TT], BF16)
    sg = Rot([c.sb("sg%d" % i, [128, TT]) for i in range(2)])
    ybuf = c.sb("y", [128, KC, TT])
    obuf = Rot([c.sb("o%d" % i, [128, KC, TT]) for i in range(2)])
    sq = c.sb("sq", [128, KC, TT], BF16)
    rstd = c.sb("rstd", [128, TT])
    ps_stat = c.ps("ps_stat", [128, 512])
    psb = Rot([c.ps("ps%d" % i, [128, 512]) for i in range(6)])
    hv = hTd.rearrange("(k p) t -> p k t", p=128)
    ov = oT.rearrange("(k p) t -> p k t", p=128)
    for i in range(T // TT):
        hb = hbuf.next()
        P.dma("sp", hb[:], hv[:, :, i * TT:(i + 1) * TT])
        emit_rstd(c, hb, KC, TT, sq, c.ones, ps_stat, rstd, D)
        for k in range(KC):
            P.tt(("dve", "pool")[k % 2], xn[:, k, :], hb[:, k, :], rstd[:, :TT], ALU.mult)
        for j in range(JC):
            pg = psb.next()
            pu = psb.next()
            for k in range(KC):
                P.mm(pg[:, :TT], Wg[:, k, j * 128:(j + 1) * 128], xn[:, k, :], start=(k == 0), stop=(k == KC - 1))
            for k in range(KC):
                P.mm(pu[:, :TT], Wu[:, k, j * 128:(j + 1) * 128], xn[:, k, :], start=(k == 0), stop=(k == KC - 1))
            s = sg.next()
            P.act(s[:, :], pg[:, :TT], AF.Silu)
            P.tt("dve", at[:, j, :], s[:, :], pu[:, :TT], ALU.mult)
        for n in range(KC):
            pb = psb.next()
            for j in range(JC):
                P.mm(pb[:, :TT], Wdn[:, j, n * 128:(n + 1) * 128], at[:, j, :], start=(j == 0), stop=(j == JC - 1))
            P.copy(("act", "dve")[n % 2], ybuf[:, n, :], pb[:, :TT])
        ob = obuf.next()
        emit_postnorm_res(c, ybuf, hb, g3, ob, TT, sq, ps_stat, rstd)
        P.dma("pool", ov[:, :, i * TT:(i + 1) * TT], ob[:])
    return c.finish()

NEG = -30000.0


def sb_consts():
    p = np.arange(128)[:, None]
    f = np.arange(512)[None, :]
    m01 = np.stack([(f > 128 * m + p) for m in range(4)]).astype(np.float32)
    lmat = (np.arange(128)[:, None] < np.arange(128)[None, :]).astype(np.float32)
    return {"m01": np.ascontiguousarray(m01.transpose(1, 0, 2)),
            "lmat": lmat, "ident": np.eye(128, dtype=np.float32)}


def load_cast(c, dst, src, stage, scale=None, chunk=2048):
    P = c.P
    n = src.shape[-1]
    i = 0
    for c0 in range(0, n, chunk):
        w = min(chunk, n - c0)
        s = stage.next()
        P.dma(("sp", "pool")[i % 2], s[:, :w], src[:, c0:c0 + w])
        if scale is not None:
            P.act(dst[:, c0:c0 + w], s[:, :w], AF.Copy, scale=scale)
        else:
            P.copy(("dve", "act")[i % 2], dst[:, c0:c0 + w], s[:, :w])
        i += 1


def build_stickbreak(S, NH=2, DH=128):
    c = Ctx()
    P = c.P
    qT = c.din("qT", [NH, DH, S])
    kT = c.din("kT", [NH, DH, S])
    vd = c.din("v", [NH, S, DH])
    m01d = c.din("m01", [128, 4, 512])
    lmd = c.din("lmat", [128, 128])
    idd = c.din("ident", [128, 128])
    oT = c.dout("oT", [NH, DH, S])
    m01 = c.sb("m01_sb", [128, 4, 512])
    negm = c.sb("negm", [128, 4, 512], BF16)
    lmat = c.sb("lmat_sb", [128, 128])
    nones = c.sb("nones", [128, 128])
    identf = c.sb("identf", [128, 128])
    ident = c.sb("ident_sb", [128, 128], BF16)
    P.dma("sp", m01[:], m01d)
    P.dma("sp", lmat[:], lmd)
    P.dma("sp", identf[:], idd)
    P.copy("dve", ident[:], identf[:])
    P.memset("dve", nones[:], -1.0)
    P.ts("dve", negm[:], m01[:], -1.0, ALU.add, s2=-NEG, op1=ALU.mult)
    stage = Rot([c.sb("stg%d" % i, [128, 2048]) for i in range(2)])
    qb = c.sb("qb", [128, S], BF16)
    kb = c.sb("kb", [128, S], BF16)
    vb = c.sb("vb", [128, S // 128, DH], BF16)
    pA = Rot([c.ps("pA%d" % i, [128, 512]) for i in range(2)])
    pB = Rot([c.ps("pB%d" % i, [128, 512]) for i in range(2)])
    pO = Rot([c.ps("pO%d" % i, [128, 512]) for i in range(2)])
    eb = Rot([c.sb("eb%d" % i, [128, 512]) for i in range(2)])
    spb = Rot([c.sb("spb%d" % i, [128, 512]) for i in range(3)])
    sps = Rot([c.sb("sps%d" % i, [128, 512]) for i in range(2)])
    atb = Rot([c.sb("atb%d" % i, [128, 512], BF16) for i in range(3)])
    ob = Rot([c.sb("ob%d" % i, [128, 512]) for i in range(2)])
    QT = 512
    for h in range(NH):
        load_cast(c, qb, qT[h], stage, scale=DH ** -0.5)
        load_cast(c, kb, kT[h], stage)
        vv = vd[h].rearrange("(n p) d -> p n d", p=128)
        for c0 in range(0, S // 128, 16):
            s = stage.next()
            sv = s[:, :].rearrange("p (n d) -> p n d", d=DH)
            nbk = min(16, S // 128 - c0)
            P.dma("sp", sv[:, :nbk, :], vv[:, c0:c0 + nbk, :])
            P.copy("dve", vb[:, c0:c0 + nbk, :], sv[:, :nbk, :])
        steps = []
        for qt in range(S // QT):
            t0 = qt * QT
            nb = (t0 + QT) // 128
            for n, kbi in enumerate(range(nb - 1, -1, -1)):
                m = (kbi * 128 - t0) // 128 if kbi * 128 >= t0 else None
                steps.append(dict(t0=t0, kb=kbi, m=m, first=(n == 0), last=(n == nb - 1)))
        st = [dict() for _ in steps]
        cur = {}

        def s12(n):
            d = steps[n]
            A = pA.next()
            qs = qb[:, d["t0"]:d["t0"] + QT]
            ks = kb[:, d["kb"] * 128:(d["kb"] + 1) * 128]
            P.mm(A[:], ks, qs)
            e = eb.next()
            P.act(e[:], A[:], AF.Exp)
            sp = spb.next()
            P.act(sp[:], e[:], AF.Ln, bias=1.0)
            if d["m"] is not None:
                P.tt("dve", sp[:], sp[:], m01[:, d["m"], :], ALU.mult)
            if d["first"]:
                ssum = sp
            else:
                ssum = sps.next()
                P.tt("pool", ssum[:], cur["ssum"][:], sp[:], ALU.add)
            cur["ssum"] = ssum
            st[n].update(sp=sp, ssum=ssum, qs=qs, ks=ks)

        def s34(n):
            d = steps[n]
            B = pB.next()
            diag = d["m"] is not None
            P.mm(B[:], st[n]["ks"], st[n]["qs"], start=True, stop=False)
            P.mm(B[:], lmat[:], st[n]["sp"][:], start=False, stop=False)
            P.mm(B[:], nones[:], st[n]["ssum"][:], start=False, stop=not diag)
            if diag:
                P.mm(B[:], ident[:], negm[:, d["m"], :], start=False, stop=True)
            at = atb.next()
            P.act(at[:], B[:], AF.Exp)
            st[n]["at"] = at

        def s5(n):
            d = steps[n]
            if d["first"]:
                cur["O"] = pO.next()
            O = cur["O"]
            P.mm(O[:], vb[:, d["kb"], :], st[n]["at"][:], start=d["first"], stop=d["last"])
            if d["last"]:
                o = ob.next()
                P.copy("dve", o[:], O[:])
                P.dma("sp", oT[h, :, d["t0"]:d["t0"] + QT], o[:])

        N = len(steps)
        for n in range(N + 2):
            if n < N:
                s12(n)
            if 0 <= n - 1 < N:
                s34(n - 1)
            if 0 <= n - 2 < N:
                s5(n - 2)
    return c.finish()

CH = 64


def hg_consts():
    j = np.arange(64)[:, None]
    i = np.arange(64)[None, :]
    return {"tri": (j <= i).astype(np.float32), "identf": np.eye(128, dtype=np.float32)}


def build_hgrn2(S, NH=2):
    c = Ctx()
    P = c.P
    qT = c.din("qT", [NH, 128, S])
    fT = c.din("fT", [NH, 128, S])
    gT = c.din("gT", [NH, 128, S])
    vd = c.din("v", [NH, S, 128])
    l0d = c.din("lb0", [128, NH])
    l1d = c.din("lb1", [128, NH])
    dnd = c.din("dn", [128, 1])
    trid = c.din("tri", [64, 64])
    idd = c.din("identf", [128, 128])
    oT = c.dout("oT", [NH, 128, S])
    NCH = S // CH
    lb = c.sb("lb_sb", [128, NH])
    oml = c.sb("oml", [128, NH])
    noml = c.sb("noml", [128, NH])
    dn = c.sb("dn_sb", [128, 1])
    tri = c.sb("tri_sb", [64, 64])
    ident = c.sb("ident_sb", [128, 128])
    onesf = c.sb("onesf", [128, 128])
    eps_t = c.sb("eps_t", [128, 1])
    l0 = c.sb("l0_sb", [128, NH])
    P.dma("sp", l0[:], l0d)
    P.dma("sp", lb[:], l1d)
    P.tt("dve", lb[:], lb[:], l0[:], ALU.subtract)
    P.act(lb[:], lb[:], AF.Sigmoid)
    P.dma("sp", dn[:], dnd)
    P.dma("sp", tri[:], trid)
    P.dma("sp", ident[:], idd)
    P.memset("dve", onesf[:], 1.0)
    P.memset("dve", eps_t[:], EPS)
    P.ts("dve", oml[:], lb[:], -1.0, ALU.mult, s2=1.0, op1=ALU.add)
    P.ts("dve", noml[:], lb[:], 1.0, ALU.mult, s2=-1.0, op1=ALU.add)
    CW = 2048 if S >= 2048 else S
    rmask = c.sb("rmask", [128, CW])
    P.memset("dve", rmask[:], 1.0)
    P.memset("dve", rmask[:, 0:CW:CH], 0.0)
    stg = Rot([c.sb("stg%d" % i, [128, CW]) for i in range(2)])
    sgn = c.sb("sgn", [128, CW])
    fg = c.sb("fg", [128, CW])
    gc = c.sb("gc", [128, CW])
    eg = c.sb("eg", [128, CW])
    eng = c.sb("eng", [128, CW])
    qs = c.sb("qs", [128, CW])
    qt = c.sb("qt", [128, S])
    kt = c.sb("kt", [128, S])
    egl = c.sb("egl", [128, NCH])
    Sst = Rot([c.sb("S%d" % i, [128, 128]) for i in range(2)])
    vbuf = Rot([c.sb("vb%d" % i, [64, 16, 128]) for i in range(2)])
    atb = Rot([c.sb("atb%d" % i, [64, 64]) for i in range(3)])
    ktb = Rot([c.sb("ktb%d" % i, [64, 128]) for i in range(3)])
    pAt = Rot([c.ps("pAt%d" % i, [128, 512]) for i in range(2)])
    pKt = Rot([c.ps("pKt%d" % i, [128, 512]) for i in range(2)])
    pO = Rot([c.ps("pO%d" % i, [128, 512]) for i in range(2)])
    pS = c.ps("pS", [128, 512])
    pN = c.ps("pN", [128, 512])
    ob = Rot([c.sb("ob%d" % i, [128, 512]) for i in range(2)])
    sqb = c.sb("sqb", [128, 512])
    rstd = c.sb("rstd", [128, 512])
    gb = Rot([c.sb("gb%d" % i, [128, 512]) for i in range(2)])
    sgb = c.sb("sgb", [128, 512])
    for h in range(NH):
        for c0 in range(0, S, CW):
            sf = stg.next()
            P.dma("sp", sf[:], fT[h, :, c0:c0 + CW])
            sq_ = stg.next()
            P.dma("pool", sq_[:], qT[h, :, c0:c0 + CW])
            P.act(sgn[:], sf[:], AF.Sigmoid, scale=-1.0)
            P.ts("dve", fg[:], sgn[:], noml[:, h:h + 1], ALU.mult, s2=1.0, op1=ALU.add)
            P.act(fg[:], fg[:], AF.Ln)
            P.op("dve", lambda e: e.tensor_tensor_scan(out=gc[:], data0=rmask[:], data1=fg[:], initial=0.0,
                                                       op0=ALU.mult, op1=ALU.add),
                 [rmask[:], fg[:]], [gc[:]])
            P.act(eg[:], gc[:], AF.Exp)
            P.act(eng[:], gc[:], AF.Exp, scale=-1.0)
            P.act(qs[:], sq_[:], AF.Silu)
            P.tt("pool", qt[:, c0:c0 + CW], qs[:], eg[:], ALU.mult)
            P.stt(kt[:, c0:c0 + CW], sgn[:], oml[:, h:h + 1], eng[:], ALU.mult, ALU.mult)
            P.copy("dve", egl[:, c0 // CH:(c0 + CW) // CH], eg[:, CH - 1:CW:CH])
        S0 = Sst.next()
        P.memset("dve", S0[:], 0.0)
        cur = {"S": S0}
        pre = {}

        def do_pre(n):
            qc = qt[:, n * CH:(n + 1) * CH]
            kc = kt[:, n * CH:(n + 1) * CH]
            pa = pAt.next()
            P.mm(pa[:64, :64], kc, qc)
            at = atb.next()
            P.tt("dve", at[:], pa[:64, :64], tri[:], ALU.mult)
            pk = pKt.next()
            P.tr(pk[:64, :128], kc, ident[:])
            ktk = ktb.next()
            P.copy("act", ktk[:], pk[:64, :128])
            pre[n] = (qc, at, ktk)

        do_pre(0)
        for n in range(NCH):
            if n + 1 < NCH:
                do_pre(n + 1)
            if n % 16 == 0:
                vb = vbuf.next()
                cur["vb"] = vb
                nck = min(16, NCH - n)
                P.dma("act", vb[:, :nck, :], vd[h, n * CH:(n + nck) * CH, :].rearrange("(n j) d -> j n d", j=CH))
            vc = cur["vb"][:, n % 16, :]
            qc, at, ktk = pre.pop(n)
            if n % 8 == 0:
                cur["O"] = pO.next()
            O = cur["O"]
            oc = O[:, (n % 8) * CH:(n % 8 + 1) * CH]
            Sc = cur["S"]
            P.mm(oc, Sc[:], qc, start=True, stop=False)
            P.mm(oc, vc, at[:], start=False, stop=True)
            P.mm(pS[:, :128], ident[:], Sc[:], start=True, stop=False)
            P.mm(pS[:, :128], ktk[:], vc, start=False, stop=True)
            Sn = Sst.next()
            P.act(Sn[:], pS[:, :128], AF.Copy, scale=egl[:, n:n + 1])
            cur["S"] = Sn
            if n % 8 == 7 or n == NCH - 1:
                t0 = (n // 8) * 512
                w = (n % 8 + 1) * CH
                o = ob.next()
                P.copy("dve", o[:, :w], O[:, :w])
                g = gb.next()
                P.dma("sp", g[:, :w], gT[h, :, t0:t0 + w])
                P.act(sqb[:, :w], o[:, :w], AF.Square)
                P.mm(pN[:, :w], onesf[:], sqb[:, :w])
                P.act(rstd[:, :w], pN[:, :w], AF.Sqrt, bias=eps_t[:, 0:1], scale=1.0 / 128)
                P.op("dve", lambda e, w=w: e.reciprocal(out=rstd[:, :w], in_=rstd[:, :w]), [rstd[:, :w]], [rstd[:, :w]])
                P.act(sgb[:, :w], g[:, :w], AF.Silu)
                P.stt(o[:, :w], o[:, :w], dn[:, 0:1], rstd[:, :w], ALU.mult, ALU.mult)
                P.tt("pool", o[:, :w], o[:, :w], sgb[:, :w], ALU.mult)
                P.dma("sp", oT[h, :, t0:t0 + w], o[:, :w])
    return c.finish()


PE_ALT = 'pool'


def gdn_consts():
    k = np.arange(64)[:, None]
    c = np.arange(64)[None, :]
    return {"tri_incl": (k <= c).astype(np.float32),
            "negs": np.where(c < k, 0.0, NEG).astype(np.float32),
            "negit": np.where(k <= c, 0.0, NEG).astype(np.float32),
            "identf": np.eye(128, dtype=np.float32)}


def build_gdn(S, NH=2, dbg=None, seg=False):
    c = Ctx()
    P = c.P
    NCH = S // CH
    HL = 3 if seg else 0
    qT = c.din("qT", [NH, 128, S + HL])
    kT = c.din("kT", [NH, 128, S + HL])
    vT = c.din("vT", [NH, 128, S + HL])
    if seg:
        Sin = c.din("S_in", [NH, 128, 128])
        Sout = c.dout("S_out", [NH, 128, 128])
    zT = c.din("zT", [NH, 128, S])
    cwd = c.din("cw", [NH, 3, 128, 4])
    acd = c.din("acol", [NH, 64, NCH])
    bcd = c.din("bcol", [NH, 64, NCH])
    ald = c.din("alog", [NH, 64, 1])
    dtd = c.din("dtb", [NH, 64, 1])
    and_ = c.din("an", [128, 1])
    trid = c.din("tri_incl", [64, 64])
    negsd = c.din("negs", [64, 64])
    negitd = c.din("negit", [64, 64])
    idd = c.din("identf", [128, 128])
    oT = c.dout("oT", [NH, 128, S])

    tri = c.sb("tri_sb", [64, 64])
    negs = c.sb("negs_sb", [64, 64])
    negit = c.sb("negit_sb", [64, 64])
    ident = c.sb("ident_sb", [128, 128])
    onesf = c.sb("onesf", [128, 128])
    eps_t = c.sb("eps_t", [128, 1])
    an = c.sb("an_sb", [128, 1])
    for (d_, s_) in ((tri, trid), (negs, negsd), (negit, negitd), (ident, idd), (an, and_)):
        P.dma("sp", d_[:], s_)
    P.memset("dve", onesf[:], 1.0)
    P.memset("dve", eps_t[:], EPS)
    id64 = ident[:64, :64]
    ones64 = onesf[:64, :64]

    CW = 2048 if S >= 2048 else S
    xbuf = Rot([c.sb("xb%d" % i, [128, CW + 3]) for i in range(2)])
    acc = c.sb("acc", [128, CW])
    tmp = c.sb("tmp", [128, CW])
    sqb = c.sb("sqb", [128, CW])
    rn = c.sb("rn", [128, 512])
    qh = c.sb("qh", [128, S])
    kh = c.sb("kh", [128, S])
    vh = c.sb("vh", [128, S])
    cw = c.sb("cw_sb", [128, 3, 4])
    gcol = c.sb("gcol", [64, NCH]); ngcol = c.sb("ngcol", [64, NCH])
    gccol = c.sb("gccol", [64, NCH]); kdcol = c.sb("kdcol", [64, NCH])
    becol = c.sb("becol", [64, NCH]); bgcol = c.sb("bgcol", [64, NCH])
    gl128 = c.sb("gl128", [128, NCH])
    t64 = c.sb("t64", [64, NCH])
    al = c.sb("al", [64, 1]); dtb = c.sb("dtb_sb", [64, 1]); negA = c.sb("negA", [64, 1])
    psT = Rot([c.ps("psT%d" % i, [128, 512]) for i in range(1)])
    psK = c.ps("psK", [128, 512])
    psX = Rot([c.ps("psX%d" % i, [128, 512]) for i in range(2)])
    psR = c.ps("psR", [128, 512])
    psO = Rot([c.ps("psO%d" % i, [128, 512]) for i in range(2)])
    psN = c.ps("psN", [128, 512])
    X0 = Rot([c.sb("X0_%d" % i, [64, 256]) for i in range(2)])
    XB = Rot([c.sb("XB%d" % i, [64, 256]) for i in range(3)])
    XF = Rot([c.sb("XF%d" % i, [64, 256]) for i in range(2)])
    PB = Rot([c.sb("PB%d" % i, [64, 64]) for i in range(3)])
    PTB = Rot([c.sb("PTB%d" % i, [64, 64]) for i in range(3)])
    kdb = Rot([c.sb("kdb%d" % i, [64, 128]) for i in range(2)])
    G1b = Rot([c.sb("G1b%d" % i, [64, 64]) for i in range(2)])
    nG1b = Rot([c.sb("nG1b%d" % i, [64, 64]) for i in range(2)])
    decSb = Rot([c.sb("decS%d" % i, [64, 64]) for i in range(2)])
    decTb = Rot([c.sb("decT%d" % i, [64, 64]) for i in range(2)])
    egbb = Rot([c.sb("egb%d" % i, [128, 64]) for i in range(2)])
    Lb = Rot([c.sb("Lb%d" % i, [64, 64]) for i in range(2)])
    LTb = Rot([c.sb("LTb%d" % i, [64, 64]) for i in range(2)])
    qkb = Rot([c.sb("qkb%d" % i, [64, 64]) for i in range(2)])
    qdb = Rot([c.sb("qdb%d" % i, [128, 64]) for i in range(2)])
    wTb = Rot([c.sb("wTb%d" % i, [128, 64]) for i in range(2)])
    vnb = Rot([c.sb("vnb%d" % i, [64, 128]) for i in range(2)])
    Sst = Rot([c.sb("S%d" % i, [128, 128]) for i in range(2)])
    Sgb = c.sb("Sgb", [128, 128])
    ob = Rot([c.sb("ob%d" % i, [128, 512]) for i in range(2)])
    zb = Rot([c.sb("zb%d" % i, [128, 512]) for i in range(2)])
    sq2 = c.sb("sq2", [128, 512])
    rstd = c.sb("rstd", [128, 512])
    sgb = c.sb("sgb", [128, 512])
    ei = [0]

    def evac(out, in_):
        ei[0] += 1
        P.copy("dve", out, in_)

    for h in range(NH):
        P.dma("sp", cw[:], cwd[h].rearrange("t p j -> p t j"))
        for ti, (src, dst) in enumerate(((qT, qh), (kT, kh), (vT, vh))):
            for c0 in range(0, S, CW):
                xb = xbuf.next()
                if seg:
                    P.dma("sp", xb[:, 0:CW + 3], src[h, :, c0:c0 + CW + 3])
                elif c0 == 0:
                    P.memset("dve", xb[:, 0:3], 0.0)
                    P.dma("sp", xb[:, 3:CW + 3], src[h, :, 0:CW])
                else:
                    P.dma("sp", xb[:, 0:CW + 3], src[h, :, c0 - 3:c0 + CW])
                P.ts("dve", acc[:], xb[:, 0:CW], cw[:, ti, 0:1], ALU.mult)
                for j in range(1, 4):
                    P.stt(acc[:], xb[:, j:j + CW], cw[:, ti, j:j + 1], acc[:], ALU.mult, ALU.add)
                if ti == 2:
                    P.act(dst[:, c0:c0 + CW], acc[:], AF.Silu)
                    continue
                P.act(tmp[:], acc[:], AF.Silu)
                P.act(sqb[:], tmp[:], AF.Square)
                for s0 in range(0, CW, 512):
                    P.mm(psN[:, :512], onesf[:], sqb[:, s0:s0 + 512])
                    P.act(rn[:], psN[:, :512], AF.Sqrt, bias=eps_t[:, 0:1], scale=1.0)
                    P.op("dve", lambda e: e.reciprocal(out=rn[:], in_=rn[:]), [rn[:]], [rn[:]])
                    sc = (128 ** -0.5) if ti == 0 else 1.0
                    P.stt(dst[:, c0 + s0:c0 + s0 + 512], tmp[:, s0:s0 + 512], sc, rn[:], ALU.mult, ALU.mult)
        P.dma("sp", t64[:], acd[h])
        P.dma("sp", becol[:], bcd[h])
        P.dma("sp", al[:], ald[h])
        P.dma("sp", dtb[:], dtd[h])
        P.act(t64[:], t64[:], AF.Exp, bias=dtb[:, 0:1])
        P.act(t64[:], t64[:], AF.Ln, bias=1.0)
        P.act(negA[:], al[:], AF.Exp)
        P.ts("dve", negA[:], negA[:], -1.0, ALU.mult)
        P.ts("dve", gcol[:], t64[:], negA[:, 0:1], ALU.mult)
        P.ts("dve", ngcol[:], gcol[:], -1.0, ALU.mult)
        P.act(becol[:], becol[:], AF.Sigmoid)
        for n0 in range(0, NCH, 512):
            nn = min(512, NCH - n0)
            P.mm(psN[:64, :nn], tri[:], gcol[:, n0:n0 + nn])
            evac(gccol[:, n0:n0 + nn], psN[:64, :nn])
            P.mm(psN[:64, :nn], ones64, gcol[:, n0:n0 + nn])
            P.tt("dve", kdcol[:, n0:n0 + nn], psN[:64, :nn], gccol[:, n0:n0 + nn], ALU.subtract)
            P.mm(psN[:, :nn], onesf[:64, :], gcol[:, n0:n0 + nn])
            P.act(gl128[:, n0:n0 + nn], psN[:, :nn], AF.Exp)
        P.act(kdcol[:], kdcol[:], AF.Exp)
        P.act(t64[:], gccol[:], AF.Exp)
        P.tt("dve", bgcol[:], becol[:], t64[:], ALU.mult)

        pre = {}

        def do_pre(n):
            sl = slice(n * CH, (n + 1) * CH)
            kc = kh[:, sl]; qc = qh[:, sl]; vc = vh[:, sl]
            pt = psT.next()
            P.tr(pt[:64, 0:128], kc, ident[:])
            P.tr(pt[:64, 128:256], vc, ident[:])
            x0 = X0.next()
            P.ts("dve", x0[:, 128:256], pt[:64, 0:128], bgcol[:, n:n + 1], ALU.mult)
            P.ts("dve", x0[:, 0:128], pt[:64, 128:256], becol[:, n:n + 1], ALU.mult)
            kd = kdb.next()
            P.ts("dve", kd[:], pt[:64, 0:128], kdcol[:, n:n + 1], ALU.mult)
            if dbg == "pre1":
                return
            G1 = G1b.next(); nG1 = nG1b.next()
            P.ts(PE_ALT, G1[:], tri[:], gcol[:, n:n + 1], ALU.mult)
            P.ts(PE_ALT, nG1[:], tri[:], ngcol[:, n:n + 1], ALU.mult)
            P.mm(psK[:64, 0:64], kc, kc)
            P.mm(psK[:64, 64:128], G1[:], ones64, start=True, stop=False)
            P.mm(psK[:64, 64:128], ones64, nG1[:], start=False, stop=False)
            P.mm(psK[:64, 64:128], id64, negs[:], start=False, stop=True)
            P.mm(psK[:64, 128:192], ones64, G1[:], start=True, stop=False)
            P.mm(psK[:64, 128:192], nG1[:], ones64, start=False, stop=False)
            P.mm(psK[:64, 128:192], id64, negit[:], start=False, stop=True)
            P.mm(psK[:64, 192:256], kc, qc)
            P.mm(psK[:, 256:320], onesf[:64, :], G1[:])
            decS = decSb.next(); decT = decTb.next(); egb = egbb.next()
            P.act(decS[:], psK[:64, 64:128], AF.Exp)
            P.act(decT[:], psK[:64, 128:192], AF.Exp)
            P.act(egb[:], psK[:, 256:320], AF.Exp)
            if dbg == "pre2":
                return
            L = Lb.next()
            P.stt(L[:], psK[:64, 0:64], becol[:, n:n + 1], decS[:], ALU.mult, ALU.mult)
            qk = qkb.next()
            P.tt("dve", qk[:], psK[:64, 192:256], decT[:], ALU.mult)
            qd = qdb.next()
            P.tt(PE_ALT, qd[:], qc, egb[:], ALU.mult)
            P.tr(pt[:64, 256:320], L[:], id64)
            LT = LTb.next()
            evac(LT[:], pt[:64, 256:320])
            if dbg == "pre3":
                return
            X = x0
            Pm, PmT = L, LT
            for lev in range(6):
                px = psX.next()
                if lev > 0:
                    P.mm(px[:64, 320:384], Pm[:], PmT[:])
                    if lev < 5:
                        P.mm(px[:64, 256:320], PmT[:], Pm[:])
                    nPT = PTB.next()
                    evac(nPT[:], px[:64, 320:384])
                    if lev < 5:
                        nP = PB.next()
                        evac(nP[:], px[:64, 256:320])
                        Pm = nP
                    PmT = nPT
                P.mm(px[:64, 0:256], PmT[:], X[:])
                Xn = XF.next() if lev == 5 else XB.next()
                P.tt("dve", Xn[:], X[:], px[:64, 0:256], ALU.subtract if lev == 0 else ALU.add)
                X = Xn
            P.tr(pt[:, 320:384], X[:, 128:256], id64)
            wT = wTb.next()
            evac(wT[:], pt[:, 320:384])
            pre[n] = dict(u=X[:, 0:128], wT=wT, qk=qk, qd=qd, kd=kd)

        S0 = Sst.next()
        if seg:
            P.dma("sp", S0[:], Sin[h])
        else:
            P.memset("dve", S0[:], 0.0)
        cur = {"S": S0}
        if dbg == "p1":
            continue
        do_pre(0)
        for n in range(NCH):
            if n + 1 < NCH:
                do_pre(n + 1)
            if dbg is not None and dbg.startswith("pre"):
                continue
            d = pre.pop(n)
            Sc = cur["S"]
            P.mm(psR[:64, 0:128], d["wT"][:], Sc[:])
            vn = vnb.next()
            P.tt("dve", vn[:], d["u"], psR[:64, 0:128], ALU.subtract)
            if n % 8 == 0:
                cur["O"] = psO.next()
            O = cur["O"]
            oc = O[:, (n % 8) * CH:(n % 8 + 1) * CH]
            P.mm(oc, Sc[:], d["qd"][:], start=True, stop=False)
            P.mm(oc, vn[:], d["qk"][:], start=False, stop=True)
            P.mm(psR[:, 128:256], d["kd"][:], vn[:])
            Sn = Sst.next()
            P.ts("dve", Sgb[:], Sc[:], gl128[:, n:n + 1], ALU.mult)
            P.tt("dve", Sn[:], Sgb[:], psR[:, 128:256], ALU.add)
            cur["S"] = Sn
            if (n % 8 == 7 or n == NCH - 1) and dbg != 'nop3':
                t0 = (n // 8) * 512
                w = (n % 8 + 1) * CH
                o = ob.next()
                P.copy("dve", o[:, :w], O[:, :w])
                if dbg == "p3a":
                    P.dma("sp", oT[h, :, t0:t0 + w], o[:, :w])
                    continue
                z = zb.next()
                P.dma("sp", z[:, :w], zT[h, :, t0:t0 + w])
                P.act(sq2[:, :w], o[:, :w], AF.Square)
                P.mm(psN[:, :w], onesf[:], sq2[:, :w])
                P.act(rstd[:, :w], psN[:, :w], AF.Sqrt, bias=eps_t[:, 0:1], scale=1.0 / 128)
                P.op("dve", lambda e, w=w: e.reciprocal(out=rstd[:, :w], in_=rstd[:, :w]), [rstd[:, :w]], [rstd[:, :w]])
                P.act(sgb[:, :w], z[:, :w], AF.Silu)
                P.stt(o[:, :w], o[:, :w], an[:, 0:1], rstd[:, :w], ALU.mult, ALU.mult)
                P.tt("dve" if dbg == "p3c" else PE_ALT, o[:, :w], o[:, :w], sgb[:, :w], ALU.mult)
                P.dma("sp", oT[h, :, t0:t0 + w], o[:, :w])
        if seg:
            P.dma("sp", Sout[h], cur["S"][:])
    return c.finish()


class _H:
    pass


def build_gdn2(S, NH=2):
    c = Ctx()
    P = c.P
    NCH = S // CH
    CW = S
    qT = c.din("qT", [NH, 128, S + 3])
    kT = c.din("kT", [NH, 128, S + 3])
    vT = c.din("vT", [NH, 128, S + 3])
    Sin = c.din("S_in", [NH, 128, 128])
    Sout = c.dout("S_out", [NH, 128, 128])
    zT = c.din("zT", [NH, 128, S])
    cwd = c.din("cw", [NH, 3, 128, 4])
    acd = c.din("acol", [NH, 64, NCH])
    bcd = c.din("bcol", [NH, 64, NCH])
    ald = c.din("alog", [NH, 64, 1])
    dtd = c.din("dtb", [NH, 64, 1])
    and_ = c.din("an", [128, 1])
    trid = c.din("tri_incl", [64, 64])
    negsd = c.din("negs", [64, 64])
    negitd = c.din("negit", [64, 64])
    idd = c.din("identf", [128, 128])
    oT = c.dout("oT", [NH, 128, S])

    tri = c.sb("tri_sb", [64, 64])
    negs = c.sb("negs_sb", [64, 64])
    negit = c.sb("negit_sb", [64, 64])
    ident = c.sb("ident_sb", [128, 128])
    onesf = c.sb("onesf", [128, 128])
    eps_t = c.sb("eps_t", [128, 1])
    an = c.sb("an_sb", [128, 1])
    for (d_, s_) in ((tri, trid), (negs, negsd), (negit, negitd), (ident, idd), (an, and_)):
        P.dma("sp", d_[:], s_)
    P.memset("dve", onesf[:], 1.0)
    P.memset("dve", eps_t[:], EPS)
    id64 = ident[:64, :64]
    ones64 = onesf[:64, :64]

    xbuf = Rot([c.sb("xb%d" % i, [128, CW + 3]) for i in range(2)])
    acc = c.sb("acc", [128, CW])
    tmp = c.sb("tmp", [128, CW])
    sqb = c.sb("sqb", [128, CW])
    rn = c.sb("rn", [128, 512])
    psT = Rot([c.ps("psT%d" % i, [128, 512]) for i in range(1)])
    psK = c.ps("psK", [128, 512])
    psX = Rot([c.ps("psX%d" % i, [128, 512]) for i in range(2)])
    psR = c.ps("psR", [128, 512])
    psO = [c.ps("psO%d" % i, [128, 512]) for i in range(2)]
    psN = c.ps("psN", [128, 512])
    NB = 2 * NH
    X0 = Rot([c.sb("X0_%d" % i, [64, 256]) for i in range(2)])
    XB = Rot([c.sb("XB%d" % i, [64, 256]) for i in range(3)])
    XF = Rot([c.sb("XF%d" % i, [64, 256]) for i in range(NB)])
    PB = Rot([c.sb("PB%d" % i, [64, 64]) for i in range(3)])
    PTB = Rot([c.sb("PTB%d" % i, [64, 64]) for i in range(3)])
    kdb = Rot([c.sb("kdb%d" % i, [64, 128]) for i in range(NB)])
    G1b = Rot([c.sb("G1b%d" % i, [64, 64]) for i in range(2)])
    nG1b = Rot([c.sb("nG1b%d" % i, [64, 64]) for i in range(2)])
    decSb = Rot([c.sb("decS%d" % i, [64, 64]) for i in range(2)])
    decTb = Rot([c.sb("decT%d" % i, [64, 64]) for i in range(2)])
    egbb = Rot([c.sb("egb%d" % i, [128, 64]) for i in range(2)])
    Lb = Rot([c.sb("Lb%d" % i, [64, 64]) for i in range(2)])
    LTb = Rot([c.sb("LTb%d" % i, [64, 64]) for i in range(2)])
    qkb = Rot([c.sb("qkb%d" % i, [64, 64]) for i in range(NB)])
    qdb = Rot([c.sb("qdb%d" % i, [128, 64]) for i in range(NB)])
    wTb = Rot([c.sb("wTb%d" % i, [128, 64]) for i in range(NB)])
    vnb = Rot([c.sb("vnb%d" % i, [64, 128]) for i in range(2)])
    Sgb = Rot([c.sb("Sgb%d" % i, [128, 128]) for i in range(2)])
    ob = Rot([c.sb("ob%d" % i, [128, 512]) for i in range(2)])
    zb = Rot([c.sb("zb%d" % i, [128, 512]) for i in range(2)])
    sq2 = c.sb("sq2", [128, 512])
    rstd = c.sb("rstd", [128, 512])
    sgb = c.sb("sgb", [128, 512])

    def evac(out, in_):
        P.copy("dve", out, in_)

    heads = []
    for h in range(NH):
        H = _H()
        H.h = h
        sfx = "_h%d" % h
        H.qh = c.sb("qh" + sfx, [128, S]); H.kh = c.sb("kh" + sfx, [128, S]); H.vh = c.sb("vh" + sfx, [128, S])
        H.cw = c.sb("cw" + sfx, [128, 3, 4])
        for nm in ("gcol", "ngcol", "gccol", "kdcol", "becol", "bgcol", "t64"):
            setattr(H, nm, c.sb(nm + sfx, [64, NCH]))
        H.gl128 = c.sb("gl128" + sfx, [128, NCH])
        H.al = c.sb("al" + sfx, [64, 1]); H.dtb = c.sb("dtb" + sfx, [64, 1]); H.negA = c.sb("negA" + sfx, [64, 1])
        H.Sst = Rot([c.sb("S%d%s" % (i, sfx), [128, 128]) for i in range(2)])
        H.O = psO[h % 2]
        H.pre = {}
        heads.append(H)

    def phase1(H):
        h = H.h
        P.dma("sp", H.cw[:], cwd[h].rearrange("t p j -> p t j"))
        for ti, (src, dst) in enumerate(((qT, H.qh), (kT, H.kh), (vT, H.vh))):
            xb = xbuf.next()
            P.dma(("sp", "act")[ti % 2], xb[:, 0:CW + 3], src[h, :, 0:CW + 3])
            P.ts("dve", acc[:], xb[:, 0:CW], H.cw[:, ti, 0:1], ALU.mult)
            for j in range(1, 4):
                P.stt(acc[:], xb[:, j:j + CW], H.cw[:, ti, j:j + 1], acc[:], ALU.mult, ALU.add)
            if ti == 2:
                P.act(dst[:, :], acc[:], AF.Silu)
                continue
            P.act(tmp[:], acc[:], AF.Silu)
            P.act(sqb[:], tmp[:], AF.Square)
            P.mm(psN[:, :CW], onesf[:], sqb[:, :])
            P.act(rn[:, :CW], psN[:, :CW], AF.Sqrt, bias=eps_t[:, 0:1], scale=1.0)
            P.op("dve", lambda e: e.reciprocal(out=rn[:, :CW], in_=rn[:, :CW]), [rn[:, :CW]], [rn[:, :CW]])
            sc = (128 ** -0.5) if ti == 0 else 1.0
            P.stt(dst[:, :], tmp[:, :], sc, rn[:, :CW], ALU.mult, ALU.mult)
        t64, becol, gcol, ngcol, gccol, kdcol, bgcol = H.t64, H.becol, H.gcol, H.ngcol, H.gccol, H.kdcol, H.bgcol
        P.dma("sp", t64[:], acd[h])
        P.dma("act", becol[:], bcd[h])
        P.dma("sp", H.al[:], ald[h])
        P.dma("act", H.dtb[:], dtd[h])
        P.act(t64[:], t64[:], AF.Exp, bias=H.dtb[:, 0:1])
        P.act(t64[:], t64[:], AF.Ln, bias=1.0)
        P.act(H.negA[:], H.al[:], AF.Exp)
        P.ts("dve", H.negA[:], H.negA[:], -1.0, ALU.mult)
        P.ts("dve", gcol[:], t64[:], H.negA[:, 0:1], ALU.mult)
        P.ts("dve", ngcol[:], gcol[:], -1.0, ALU.mult)
        P.act(becol[:], becol[:], AF.Sigmoid)
        P.mm(psN[:64, :NCH], tri[:], gcol[:, :])
        evac(gccol[:, :], psN[:64, :NCH])
        P.mm(psN[:64, :NCH], ones64, gcol[:, :])
        P.tt("dve", kdcol[:, :], psN[:64, :NCH], gccol[:, :], ALU.subtract)
        P.mm(psN[:, :NCH], onesf[:64, :], gcol[:, :])
        P.act(H.gl128[:, :], psN[:, :NCH], AF.Exp)
        P.act(kdcol[:], kdcol[:], AF.Exp)
        P.act(t64[:], gccol[:], AF.Exp)
        P.tt("dve", bgcol[:], becol[:], t64[:], ALU.mult)
        S0 = H.Sst.next()
        P.dma("sp", S0[:], Sin[h])
        H.S = S0

    def do_pre(H, n):
        sl = slice(n * CH, (n + 1) * CH)
        kc = H.kh[:, sl]; qc = H.qh[:, sl]; vc = H.vh[:, sl]
        pt = psT.next()
        P.tr(pt[:64, 0:128], kc, ident[:])
        P.tr(pt[:64, 128:256], vc, ident[:])
        x0 = X0.next()
        P.ts("dve", x0[:, 128:256], pt[:64, 0:128], H.bgcol[:, n:n + 1], ALU.mult)
        P.ts("dve", x0[:, 0:128], pt[:64, 128:256], H.becol[:, n:n + 1], ALU.mult)
        kd = kdb.next()
        P.ts("dve", kd[:], pt[:64, 0:128], H.kdcol[:, n:n + 1], ALU.mult)
        G1 = G1b.next(); nG1 = nG1b.next()
        P.ts("pool", G1[:], tri[:], H.gcol[:, n:n + 1], ALU.mult)
        P.ts("pool", nG1[:], tri[:], H.ngcol[:, n:n + 1], ALU.mult)
        P.mm(psK[:64, 0:64], kc, kc)
        P.mm(psK[:64, 64:128], G1[:], ones64, start=True, stop=False)
        P.mm(psK[:64, 64:128], ones64, nG1[:], start=False, stop=False)
        P.mm(psK[:64, 64:128], id64, negs[:], start=False, stop=True)
        P.mm(psK[:64, 128:192], ones64, G1[:], start=True, stop=False)
        P.mm(psK[:64, 128:192], nG1[:], ones64, start=False, stop=False)
        P.mm(psK[:64, 128:192], id64, negit[:], start=False, stop=True)
        P.mm(psK[:64, 192:256], kc, qc)
        P.mm(psK[:, 256:320], onesf[:64, :], G1[:])
        decS = decSb.next(); decT = decTb.next(); egb = egbb.next()
        P.act(decS[:], psK[:64, 64:128], AF.Exp)
        P.act(decT[:], psK[:64, 128:192], AF.Exp)
        P.act(egb[:], psK[:, 256:320], AF.Exp)
        L = Lb.next()
        P.stt(L[:], psK[:64, 0:64], H.becol[:, n:n + 1], decS[:], ALU.mult, ALU.mult)
        qk = qkb.next()
        P.tt("dve", qk[:], psK[:64, 192:256], decT[:], ALU.mult)
        qd = qdb.next()
        P.tt("pool", qd[:], qc, egb[:], ALU.mult)
        P.tr(pt[:64, 256:320], L[:], id64)
        LT = LTb.next()
        evac(LT[:], pt[:64, 256:320])
        X = x0
        Pm, PmT = L, LT
        for lev in range(6):
            px = psX.next()
            if lev > 0:
                P.mm(px[:64, 320:384], Pm[:], PmT[:])
                if lev < 5:
                    P.mm(px[:64, 256:320], PmT[:], Pm[:])
                nPT = PTB.next()
                evac(nPT[:], px[:64, 320:384])
                if lev < 5:
                    nP = PB.next()
                    evac(nP[:], px[:64, 256:320])
                    Pm = nP
                PmT = nPT
            P.mm(px[:64, 0:256], PmT[:], X[:])
            Xn = XF.next() if lev == 5 else XB.next()
            P.tt("dve", Xn[:], X[:], px[:64, 0:256], ALU.subtract if lev == 0 else ALU.add)
            X = Xn
        P.tr(pt[:, 320:384], X[:, 128:256], id64)
        wT = wTb.next()
        evac(wT[:], pt[:, 320:384])
        H.pre[n] = dict(u=X[:, 0:128], wT=wT, qk=qk, qd=qd, kd=kd)

    def rec(H, n):
        d = H.pre.pop(n)
        Sc = H.S
        P.mm(psR[:64, 0:128], d["wT"][:], Sc[:])
        vn = vnb.next()
        P.tt("dve", vn[:], d["u"], psR[:64, 0:128], ALU.subtract)
        oc = H.O[:, n * CH:(n + 1) * CH]
        P.mm(oc, Sc[:], d["qd"][:], start=True, stop=False)
        P.mm(oc, vn[:], d["qk"][:], start=False, stop=True)
        P.mm(psR[:, 128:256], d["kd"][:], vn[:])
        Sn = H.Sst.next()
        sg_ = Sgb.next()
        P.ts("dve", sg_[:], Sc[:], H.gl128[:, n:n + 1], ALU.mult)
        P.tt("dve", Sn[:], sg_[:], psR[:, 128:256], ALU.add)
        H.S = Sn

    def phase3(H):
        h = H.h
        w = S
        o = ob.next()
        P.copy("dve", o[:, :w], H.O[:, :w])
        z = zb.next()
        P.dma("act", z[:, :w], zT[h, :, 0:w])
        P.act(sq2[:, :w], o[:, :w], AF.Square)
        P.mm(psN[:, :w], onesf[:], sq2[:, :w])
        P.act(rstd[:, :w], psN[:, :w], AF.Sqrt, bias=eps_t[:, 0:1], scale=1.0 / 128)
        P.op("dve", lambda e: e.reciprocal(out=rstd[:, :w], in_=rstd[:, :w]), [rstd[:, :w]], [rstd[:, :w]])
        P.act(sgb[:, :w], z[:, :w], AF.Silu)
        P.stt(o[:, :w], o[:, :w], an[:, 0:1], rstd[:, :w], ALU.mult, ALU.mult)
        P.tt("pool", o[:, :w], o[:, :w], sgb[:, :w], ALU.mult)
        P.dma("sp", oT[h, :, 0:w], o[:, :w])
        P.dma("act", Sout[h], H.S[:])

    for H in heads:
        phase1(H)
    for H in heads:
        do_pre(H, 0)
    for n in range(NCH):
        if n + 1 < NCH:
            for H in heads:
                do_pre(H, n + 1)
        for H in heads:
            rec(H, n)
    for H in heads:
        phase3(H)
    return c.finish()

TOPK = 256
NIT = 16


def t5_bucket_np(rel):
    nb = 16
    max_exact = 8
    ret = np.where(rel > 0, nb, 0)
    n = np.abs(rel)
    large = max_exact + (np.log(np.maximum(n, 1).astype(np.float32) / np.float32(max_exact))
                         / np.float32(math.log(128 / max_exact)) * np.float32(nb - max_exact)).astype(np.int32)
    large = np.minimum(large, nb - 1)
    return ret + np.where(n < max_exact, n, large)


def dsa_consts(r, nblk):
    t = np.arange(128)[:, None]
    cc = np.arange(384)[None, :]
    rel = cc - 128 - 128 * r - t
    bk = t5_bucket_np(rel).astype(np.float32)
    q0rel = 128 * r + t
    lim = ((q0rel // 64) + 1) * 64
    return {"bk": bk, "limrel": np.ascontiguousarray(np.broadcast_to(lim, (128, 1))).astype(np.float32),
            "iota256": np.ascontiguousarray(np.broadcast_to(np.arange(256, dtype=np.float32)[None, :], (128, 256))),
            "identf": np.eye(128, dtype=np.float32)}


def build_dsa(S, NHB=4, NIH=8, DI=64):
    c = Ctx()
    P = c.P
    SQ = S // 2
    NBLK = SQ // 128
    qbT = c.din("qbT", [NHB, 128, SQ])
    kbT = c.din("kbT", [NHB, 128, S])
    vbd = c.din("vb", [S, NHB * 128])
    qiT = c.din("qiT", [NIH, DI, SQ])
    kiT = c.din("kiT", [DI, S])
    wid = c.din("wi", [128, NBLK, NIH])
    bkd = c.din("bk", [128, 384])
    limd = c.din("limrel", [128, 1])
    iod = c.din("iota256", [128, 256])
    idd = c.din("identf", [128, 128])
    reld = c.din("rel", [1, 32 * NHB])
    oD = c.dout("o", [SQ, NHB * 128])

    ident = c.sb("ident_sb", [128, 128])
    identb = c.sb("identb", [128, 128], BF16)
    bk = c.sb("bk_sb", [128, 384])
    lim = c.sb("lim_sb", [128, 1])
    io = c.sb("io_sb", [128, 256])
    tbl = c.sb("tbl", [128, 32 * NHB])
    tblm = c.sb("tblm", [128, 32 * NHB])
    wi = c.sb("wi_sb", [128, NBLK, NIH])
    for (d_, s_) in ((ident, idd), (bk, bkd), (lim, limd), (io, iod), (wi, wid)):
        P.dma("sp", d_[:], s_)
    P.dma("sp", tbl[:], reld[0:1, :].partition_broadcast(128))
    P.copy("dve", identb[:], ident[:])
    tv = tbl[:, :].rearrange("p (b h) -> p b h", h=NHB)
    tmv = tblm[:, :].rearrange("p (b h) -> p b h", h=NHB)
    for h in range(NHB):
        P.ts("dve", tmv[:, :, h], tv[:, :, h], tbl[:, 15 * NHB + h:15 * NHB + h + 1], ALU.subtract)
    bw = c.sb("bw", [128, NHB, 384])
    btmp = c.sb("btmp", [128, 384])
    for h in range(NHB):
        P.memset("dve", bw[:, h, :], 0.0)
        for b in range(32):
            if b == 15:
                continue
            P.ts("dve", btmp[:], bk[:], float(b), ALU.is_equal, s2=tblm[:, b * NHB + h:b * NHB + h + 1], op1=ALU.mult)
            P.tt("dve", bw[:, h, :], bw[:, h, :], btmp[:], ALU.add)

    NHS = 2
    stage = Rot([c.sb("stg%d" % i, [128, 2048]) for i in range(2)])
    kb = c.sb("kb", [128, NHS, S], BF16)
    vb = c.sb("vb_sb", [128, S // 128, NHS * 128], BF16)
    ki = c.sb("ki", [DI, S], BF16)
    scb = c.sb("scb", [128, S])
    mb = c.sb("mb", [128, S], BF16)
    pb = c.sb("pb", [128, S], BF16)
    thr_all = c.sb("thr_all", [128, NBLK])
    qis = Rot([c.sb("qis%d" % i, [DI, NIH, 128]) for i in range(2)])
    qib = Rot([c.sb("qib%d" % i, [DI, NIH, 128], BF16) for i in range(2)])
    qbs = Rot([c.sb("qbs%d" % i, [128, NHS, 128]) for i in range(2)])
    qbb = Rot([c.sb("qbb%d" % i, [128, NHS, 128], BF16) for i in range(2)])
    rl = Rot([c.sb("rl%d" % i, [128, 512]) for i in range(3)])
    pen = c.sb("pen", [128, 256])
    sm = {k: c.sb("sm_" + k, [128, 1]) for k in ("lo", "hi", "mid", "nmid", "cnt", "ssum", "tot", "sel", "d", "e", "amax", "rmax", "nmax", "rsum", "rinv")}
    pI = Rot([c.ps("pI%d" % i, [128, 512]) for i in range(2)])
    pL = Rot([c.ps("pL%d" % i, [128, 512]) for i in range(2)])
    pT = Rot([c.ps("pT%d" % i, [128, 512], BF16) for i in range(2)])
    pO = c.ps("pO", [128, 512])
    pts = Rot([c.sb("pts%d" % i, [128, 512], BF16) for i in range(3)])
    osb = Rot([c.sb("osb%d" % i, [128, 128]) for i in range(2)])

    load_cast(c, ki, kiT, Rot([stage.bufs[0][:DI, :], stage.bufs[1][:DI, :]]))

    for sweep in range(NHB // NHS):
        h0 = sweep * NHS
        for hh in range(NHS):
            load_cast(c, kb[:, hh, :], kbT[h0 + hh], stage)
        vv = vbd.rearrange("(n p) d -> p n d", p=128)
        for n0 in range(0, S // 128, 8):
            s = stage.next()
            sv = s[:, :].rearrange("p (n d) -> p n d", d=NHS * 128)
            P.dma("sp", sv[:, :8, :], vv[:, n0:n0 + 8, h0 * 128:(h0 + NHS) * 128])
            P.copy("pool", vb[:, n0:n0 + 8, :], sv[:, :8, :])
        for i in range(NBLK):
            KR = (2 * i + 2) * 128
            tsl = slice(i * 128, (i + 1) * 128)
            qs_ = qis.next()
            P.dma("act", qs_[:], qiT[:, :, tsl].rearrange("h d t -> d h t"))
            qi = qib.next()
            P.copy("pool", qi[:], qs_[:])
            qb_ = qbs.next()
            P.dma("act", qb_[:], qbT[h0:h0 + NHS, :, tsl].rearrange("h d t -> d h t"))
            qb = qbb.next()
            P.act(qb[:], qb_[:], AF.Copy, scale=128 ** -0.5)
            for s0 in range(0, KR, 512):
                w = min(512, KR - s0)
                for ih in range(NIH):
                    ps = pI.next()
                    P.mm(ps[:, :w], qi[:, ih, :], ki[:, s0:s0 + w])
                    r_ = rl.next()
                    P.act(r_[:, :w], ps[:, :w], AF.Relu)
                    if ih == 0:
                        P.ts("dve", scb[:, s0:s0 + w], r_[:, :w], wi[:, i, ih:ih + 1], ALU.mult)
                    else:
                        P.stt(scb[:, s0:s0 + w], r_[:, :w], wi[:, i, ih:ih + 1], scb[:, s0:s0 + w], ALU.mult, ALU.add)
            P.ts("dve", pen[:], io[:], lim[:, 0:1], ALU.is_ge, s2=NEG, op1=ALU.mult)
            if sweep == 0:
                if i >= 1:
                    P.op("dve", lambda e, KR=KR: e.tensor_reduce(out=sm["amax"][:], in_=scb[:, :KR], axis=AX.X, op=ALU.max, apply_absolute_value=True),
                         [scb[:, :KR]], [sm["amax"][:]])
                P.tt("dve", scb[:, KR - 256:KR], scb[:, KR - 256:KR], pen[:], ALU.add)
                if i >= 1:
                    P.ts("dve", sm["hi"][:], sm["amax"][:], 1.0, ALU.add)
                    P.ts("dve", sm["lo"][:], sm["amax"][:], -1.0, ALU.mult, s2=-1.0, op1=ALU.add)
                    KD = max(128, int(round(KR * 0.45 / 128)) * 128)
                    NA = KR - KD
                    for it in range(NIT):
                        P.ts("dve", sm["mid"][:], sm["lo"][:], sm["hi"][:, 0:1], ALU.add, s2=0.5, op1=ALU.mult)
                        P.ts("dve", sm["nmid"][:], sm["mid"][:], -1.0, ALU.mult)
                        P.act(mb[:, KD:KR], scb[:, KD:KR], AF.Sign, bias=sm["nmid"][:, 0:1], accum_out=sm["ssum"][:])
                        P.tt("dve", sm["d"][:], sm["mid"][:], sm["lo"][:], ALU.subtract)
                        P.tt("dve", sm["e"][:], sm["hi"][:], sm["mid"][:], ALU.subtract)
                        P.ts("dve", pb[:, :KD], scb[:, :KD], sm["mid"][:, 0:1], ALU.is_ge, s2=0.0, op1=ALU.add,
                             accum_out=sm["cnt"][:])
                        P.stt(sm["tot"][:], sm["cnt"][:], 2.0, sm["ssum"][:], ALU.mult, ALU.add)
                        P.ts("dve", sm["sel"][:], sm["tot"][:], float(2 * TOPK - NA) - 0.5, ALU.is_ge)
                        P.stt(sm["lo"][:], sm["d"][:], sm["sel"][:, 0:1], sm["lo"][:], ALU.mult, ALU.add)
                        P.stt(sm["hi"][:], sm["e"][:], sm["sel"][:, 0:1], sm["mid"][:], ALU.mult, ALU.add)
                    P.copy("dve", thr_all[:, i:i + 1], sm["lo"][:])
                else:
                    P.memset("dve", thr_all[:, i:i + 1], -10000.0)
            else:
                P.tt("dve", scb[:, KR - 256:KR], scb[:, KR - 256:KR], pen[:], ALU.add)
            P.ts("dve", mb[:, :KR], scb[:, :KR], thr_all[:, i:i + 1], ALU.is_lt, s2=NEG, op1=ALU.mult)
            for hh in range(NHS):
                h = h0 + hh
                for s0 in range(0, KR, 512):
                    w = min(512, KR - s0)
                    ps = pL.next()
                    P.mm(ps[:, :w], qb[:, hh, :], kb[:, hh, s0:s0 + w])
                    P.tt("dve", scb[:, s0:s0 + w], ps[:, :w], mb[:, s0:s0 + w], ALU.add)
                if i == 0:
                    P.tt("pool", scb[:, 0:256], scb[:, 0:256], bw[:, h, 128:384], ALU.add)
                else:
                    P.tt("pool", scb[:, KR - 384:KR], scb[:, KR - 384:KR], bw[:, h, :], ALU.add)
                P.op("dve", lambda e, KR=KR: e.tensor_reduce(out=sm["rmax"][:], in_=scb[:, :KR], axis=AX.X, op=ALU.max),
                     [scb[:, :KR]], [sm["rmax"][:]])
                P.ts("dve", sm["nmax"][:], sm["rmax"][:], -1.0, ALU.mult)
                P.act(pb[:, :KR], scb[:, :KR], AF.Exp, bias=sm["nmax"][:, 0:1], accum_out=sm["rsum"][:])
                nkb = KR // 128
                for k0 in range(0, nkb, 4):
                    kn = min(4, nkb - k0)
                    pt = pT.next()
                    for k in range(kn):
                        P.tr(pt[:, k * 128:(k + 1) * 128], pb[:, (k0 + k) * 128:(k0 + k + 1) * 128], identb[:])
                    pt_s = pts.next()
                    P.copy("act", pt_s[:, :kn * 128], pt[:, :kn * 128])
                    for k in range(kn):
                        P.mm(pO[:, :128], pt_s[:, k * 128:(k + 1) * 128], vb[:, k0 + k, hh * 128:(hh + 1) * 128],
                             start=(k0 + k == 0), stop=(k0 + k == nkb - 1))
                P.op("dve", lambda e: e.reciprocal(out=sm["rinv"][:], in_=sm["rsum"][:]), [sm["rsum"][:]], [sm["rinv"][:]])
                o_ = osb.next()
                P.ts("dve", o_[:], pO[:, :128], sm["rinv"][:, 0:1], ALU.mult)
                P.dma("sp", oD[tsl, h * 128:(h + 1) * 128], o_[:])
    return c.finish()
from concourse.bass_utils import run_bass_kernel_spmd

NCORE = 8
B_, S_, DFF_ = 4, 8192, 2816
EVEN_N, ODD_N = 4176, 3584


def _run(nc, in_maps):
    res = run_bass_kernel_spmd(nc, in_maps, core_ids=list(range(NCORE)))
    return res.results


def _gl(g):
    return np.ascontiguousarray(g.reshape(KC, 128).T)


def _C(a):
    return np.ascontiguousarray(a, dtype=np.float32)


def kernel(x, norm_g, w_in_even, conv_w_even, a_log_even, dt_bias_even, a_norm_even, w_out_even, rel_bias,
           w_in_odd, lb_logits, d_norm_odd, w_out_odd, w_gate, w_up, w_down):
    f32 = np.float32
    x = np.asarray(x, f32)
    NT = B_ * S_
    TPC = NT // NCORE
    S = S_
    hT = _C(x.reshape(NT, D).T)

    def tok(c):
        return slice(c * TPC, (c + 1) * TPC)

    def dense_inproj(hT, g, W):
        N = W.shape[1]
        nc = build_inproj(TPC, N)
        r = _run(nc, [{"xT": _C(hT[:, tok(c)]), "g": _gl(g), "W": _C(W)} for c in range(NCORE)])
        return np.concatenate([r[c]["pT"] for c in range(NCORE)], axis=1)

    def dense_out(catT, hT, g, W):
        nc = build_outproj(TPC)
        r = _run(nc, [{"catT": _C(catT[:, tok(c)]), "hT": _C(hT[:, tok(c)]), "g": _gl(g), "W": _C(W)} for c in range(NCORE)])
        return np.concatenate([r[c]["oT"] for c in range(NCORE)], axis=1)

    def dense_ffn(hT, g2, g3, Wg, Wu, Wd):
        nc = build_ffn(TPC, DFF_)
        r = _run(nc, [{"hT": _C(hT[:, tok(c)]), "g2": _gl(g2), "g3": _gl(g3), "Wg": _C(Wg), "Wu": _C(Wu), "Wd": _C(Wd)}
                      for c in range(NCORE)])
        return np.concatenate([r[c]["oT"] for c in range(NCORE)], axis=1)

    P0 = dense_inproj(hT, norm_g[0, 0], w_in_even[0])
    catT = np.empty((D, NT), f32)
    SEG = 512
    NCHS = SEG // 64
    cwv = np.asarray(conv_w_even[0], f32)
    gc = gdn_consts()
    state = [np.zeros((2, 128, 128), f32) for _ in range(NCORE)]

    def halo(rows, b, s0):
        out = np.zeros((128, SEG + 3), f32)
        lo = max(0, s0 - 3)
        out[:, 3 - (s0 - lo):] = P0[rows, b * S + lo:b * S + s0 + SEG]
        return out

    for sg in range(S // SEG):
        s0 = sg * SEG
        maps = []
        for c in range(NCORE):
            b, hp = c // 2, c % 2
            cols = slice(b * S + s0, b * S + s0 + SEG)
            hs = [2 * hp, 2 * hp + 1]
            m = {"qT": np.stack([halo(slice(h * 128, (h + 1) * 128), b, s0) for h in hs]),
                 "kT": np.stack([halo(slice(512 + h * 128, 512 + (h + 1) * 128), b, s0) for h in hs]),
                 "vT": np.stack([halo(slice(1024 + h * 128, 1024 + (h + 1) * 128), b, s0) for h in hs]),
                 "zT": _C(np.stack([P0[1536 + h * 128:1536 + (h + 1) * 128, cols] for h in hs])),
                 "cw": _C(np.stack([np.stack([cwv[:, t * 512 + h * 128:t * 512 + (h + 1) * 128].T for t in range(3)]) for h in hs])),
                 "acol": _C(np.stack([P0[2048 + h, cols].reshape(NCHS, 64).T for h in hs])),
                 "bcol": _C(np.stack([P0[2052 + h, cols].reshape(NCHS, 64).T for h in hs])),
                 "alog": _C(np.stack([np.broadcast_to(np.asarray(a_log_even, f32)[0, h], (64, 1)) for h in hs])),
                 "dtb": _C(np.stack([np.broadcast_to(np.asarray(dt_bias_even, f32)[0, h], (64, 1)) for h in hs])),
                 "an": _C(np.asarray(a_norm_even, f32)[0].reshape(128, 1)),
                 "S_in": _C(state[c])}
            m.update(gc)
            maps.append(m)
        r = _run(build_gdn2(SEG, 2), maps)
        for c in range(NCORE):
            b, hp = c // 2, c % 2
            state[c] = r[c]["S_out"]
            for j in range(2):
                h = 2 * hp + j
                catT[h * 128:(h + 1) * 128, b * S + s0:b * S + s0 + SEG] = r[c]["oT"][j]
    NBLK = S // 256
    maps = []
    toks = []
    for c in range(NCORE):
        b, par = c // 2, c % 2
        blocks = [2 * i + par for i in range(NBLK)]
        tk = np.concatenate([np.arange(bk_ * 128, (bk_ + 1) * 128) for bk_ in blocks])
        toks.append(tk)
        cols = slice(b * S, (b + 1) * S)
        Pb = P0[:, cols]
        m = {"qbT": _C(Pb[2056:2568][:, tk].reshape(4, 128, -1)),
             "kbT": _C(Pb[2568:3080].reshape(4, 128, S)),
             "vb": _C(Pb[3080:3592].T),
             "qiT": _C(Pb[3592:4104][:, tk].reshape(8, 64, -1)),
             "kiT": _C(Pb[4104:4168]),
             "wi": _C(Pb[4168:4176][:, tk].T.reshape(NBLK, 128, 8).transpose(1, 0, 2)),
             "rel": _C(np.asarray(rel_bias, f32).reshape(1, 128))}
        m.update(dsa_consts(par, NBLK))
        maps.append(m)
    r = _run(build_dsa(S), maps)
    for c in range(NCORE):
        b = c // 2
        catT[512:1024, b * S + toks[c]] = r[c]["o"].T
    hT = dense_out(catT, hT, norm_g[0, 1], w_out_even[0])
    hT = dense_ffn(hT, norm_g[0, 2], norm_g[0, 3], w_gate[0], w_up[0], w_down[0])

    P1 = dense_inproj(hT, norm_g[1, 0], w_in_odd[0])
    catT = np.empty((D, NT), f32)
    sc_ = sb_consts()
    maps = []
    for c in range(NCORE):
        b, hp = c // 2, c % 2
        cols = slice(b * S, (b + 1) * S)
        hs = [2 * hp, 2 * hp + 1]
        m = {"qT": _C(np.stack([P1[h * 128:(h + 1) * 128, cols] for h in hs])),
             "kT": _C(np.stack([P1[512 + h * 128:512 + (h + 1) * 128, cols] for h in hs])),
             "v": _C(np.stack([P1[1024 + h * 128:1024 + (h + 1) * 128, cols].T for h in hs]))}
        m.update(sc_)
        maps.append(m)
    r = _run(build_stickbreak(S, 2), maps)
    for c in range(NCORE):
        b, hp = c // 2, c % 2
        for j in range(2):
            h = 2 * hp + j
            catT[h * 128:(h + 1) * 128, b * S:(b + 1) * S] = r[c]["oT"][j]
    hc = hg_consts()
    lbl = np.asarray(lb_logits, f32).reshape(2, 4, 128)
    maps = []
    for c in range(NCORE):
        b, hp = c // 2, c % 2
        cols = slice(b * S, (b + 1) * S)
        hs = [2 * hp, 2 * hp + 1]
        m = {"qT": _C(np.stack([P1[1536 + h * 128:1536 + (h + 1) * 128, cols] for h in hs])),
             "fT": _C(np.stack([P1[2048 + h * 128:2048 + (h + 1) * 128, cols] for h in hs])),
             "v": _C(np.stack([P1[2560 + h * 128:2560 + (h + 1) * 128, cols].T for h in hs])),
             "gT": _C(np.stack([P1[3072 + h * 128:3072 + (h + 1) * 128, cols] for h in hs])),
             "lb0": _C(np.stack([lbl[0, h] for h in hs], axis=1)),
             "lb1": _C(np.stack([lbl[1, h] for h in hs], axis=1)),
             "dn": _C(np.asarray(d_norm_odd, f32)[0].reshape(128, 1))}
        m.update(hc)
        maps.append(m)
    r = _run(build_hgrn2(S, 2), maps)
    for c in range(NCORE):
        b, hp = c // 2, c % 2
        for j in range(2):
            h = 2 * hp + j
            catT[512 + h * 128:512 + (h + 1) * 128, b * S:(b + 1) * S] = r[c]["oT"][j]
    hT = dense_out(catT, hT, norm_g[1, 1], w_out_odd[0])
    hT = dense_ffn(hT, norm_g[1, 2], norm_g[1, 3], w_gate[1], w_up[1], w_down[1])
    return np.ascontiguousarray(hT.T).reshape(B_, S_, D).astype(f32)
```
